# Optimizing a Trainium2 kernel written in Bass

```python
import jax, jax.numpy as jnp
from jax import lax
import numpy as np

D_MODEL = 1024
BATCH = 2
SEQ = 8192
DEPTH = 1

GRID_W = 64
CTX_LEN = 256
GLA_HEADS = 4
GLA_DK = D_MODEL // 2
GLA_DV = D_MODEL
GLA_HK = GLA_DK // GLA_HEADS
GLA_HV = GLA_DV // GLA_HEADS
GLA_RANK = 16
GLA_GATE_NORM = 16.0
GLA_CHUNK = 64
CONV_CH = D_MODEL
CONV_K = 3
N_EXPERTS = 16
EC_CAPACITY = 2
D_EXPERT = 2 * D_MODEL
N_MOD = 6
EPS = 1e-6
P_IN = 2 * GLA_DK + 2 * GLA_DV + 2 * GLA_RANK + 3 * CONV_CH + 2 * D_MODEL

kernel_name = "hybrid_gla_shortconv_ecmoe_dit"


def rms_norm(x, g):
    xf = x.astype(jnp.float32)
    y = xf * lax.rsqrt(jnp.mean(xf * xf, axis=-1, keepdims=True) + EPS)
    return (y * g.astype(jnp.float32)).astype(x.dtype)


def adaln(cond, w_ada, b_ada):
    mod = jax.nn.silu(cond) @ w_ada + b_ada
    return [m[:, None, :] for m in jnp.split(mod, N_MOD, axis=-1)]


def modulate(h, shift, scale):
    return h * (1.0 + scale) + shift


def to_heads(t, hd):
    return t.reshape(t.shape[0], t.shape[1], GLA_HEADS, hd).transpose(0, 2, 1, 3)


def project(h, w_in, w_a_up, b_a):
    z = h @ w_in
    offs = np.cumsum([GLA_DK, GLA_DK, GLA_DV, GLA_DV, GLA_RANK, GLA_RANK,
                      CONV_CH, CONV_CH, CONV_CH, D_MODEL])
    q, k, v, g, lr_f, lr_b, u, bg, cg, r_gla, r_conv = jnp.split(z, offs, axis=-1)
    la_f = jax.nn.log_sigmoid(lr_f @ w_a_up[0] + b_a[0]) / GLA_GATE_NORM
    la_b = jax.nn.log_sigmoid(lr_b @ w_a_up[1] + b_a[1]) / GLA_GATE_NORM
    q = to_heads(q * (GLA_HK ** -0.5), GLA_HK)
    return (q, to_heads(k, GLA_HK), to_heads(v, GLA_HV), g,
            to_heads(la_f, GLA_HK), to_heads(la_b, GLA_HK), u, bg, cg, r_gla, r_conv)


def gla_chunked(q, k, v, log_a, s0):
    bsz, h, n, _ = q.shape
    dv = v.shape[-1]
    nc = n // GLA_CHUNK
    rs = lambda t: t.astype(jnp.float32).reshape(bsz, h, nc, GLA_CHUNK, t.shape[-1])
    q, k, v, log_a = rs(q), rs(k), rs(v), rs(log_a)
    b = jnp.cumsum(log_a, axis=-2)
    b_last = b[..., -1:, :]
    q_in = q * jnp.exp(b)
    k_in = k * jnp.exp(-b)
    k_dec = k * jnp.exp(b_last - b)
    mask = jnp.tril(jnp.ones((GLA_CHUNK, GLA_CHUNK), dtype=bool))
    scores = jnp.where(mask, jnp.einsum('bhcld,bhcmd->bhclm', q_in, k_in), 0.0)
    o_intra = jnp.einsum('bhclm,bhcme->bhcle', scores, v)
    u = jnp.einsum('bhcld,bhcle->bhcde', k_dec, v)
    decay = jnp.exp(b_last[..., 0, :])

    def step(s, inp):
        dec_c, u_c = inp
        return dec_c[..., None] * s + u_c, s

    s_fin, s_prev = lax.scan(step, s0.astype(jnp.float32),
                             (jnp.moveaxis(decay, 2, 0), jnp.moveaxis(u, 2, 0)))
    s_prev = jnp.moveaxis(s_prev, 0, 2)
    o = o_intra + jnp.einsum('bhcld,bhcde->bhcle', q_in, s_prev)
    return o.reshape(bsz, h, n, dv), s_fin


def gla_final_state(k, v, log_a):
    k, v, log_a = k.astype(jnp.float32), v.astype(jnp.float32), log_a.astype(jnp.float32)
    b = jnp.cumsum(log_a, axis=2)
    return jnp.einsum('bhnd,bhne->bhde', k * jnp.exp(b[:, :, -1:, :] - b), v)


def centred_conv3(u, w):
    up = jnp.pad(u, [(0, 0)] * (u.ndim - 2) + [(1, 1), (0, 0)])
    return up[..., :-2, :] * w[0] + up[..., 1:-1, :] * w[1] + up[..., 2:, :] * w[2]


def flip_seq(t):
    return jnp.flip(t, axis=2)


def token_mixer(proj, grid_rows, s0_f, s0_b, gla_norm_g, gla_w_o, conv_w, conv_w_out, merge_w_out):
    q, k, v, g, la_f, la_b, u, bg, cg, r_gla, r_conv = proj
    bsz, n, _ = u.shape
    dt = u.dtype
    o_f, s_f = gla_chunked(q, k, v, la_f, s0_f)
    o_b, s_b = gla_chunked(flip_seq(q), flip_seq(k), flip_seq(v), flip_seq(la_b), s0_b)
    o = o_f + flip_seq(o_b)
    o = rms_norm(o, gla_norm_g).transpose(0, 2, 1, 3).reshape(bsz, n, GLA_DV).astype(dt)
    y_gla = (o * jax.nn.silu(g)) @ gla_w_o
    cu = cg * u
    if grid_rows is None:
        conv = centred_conv3(cu, conv_w)
    else:
        conv = centred_conv3(cu.reshape(bsz, grid_rows, GRID_W, CONV_CH), conv_w).reshape(bsz, n, CONV_CH)
    y_conv = (bg * conv) @ conv_w_out
    merged = jax.nn.sigmoid(r_gla) * y_gla + jax.nn.sigmoid(r_conv) * y_conv
    return merged @ merge_w_out, s_f, s_b


def context_states(proj):
    _, k, v, _, la_f, la_b = proj[:6]
    s_f = gla_final_state(k, v, la_f)
    s_b = gla_final_state(flip_seq(k), flip_seq(v), flip_seq(la_b))
    return s_f, s_b


def expert_choice_ffn(h, w_router, w_gate, w_up, w_down):
    bsz, n, d = h.shape
    cap = EC_CAPACITY * n // N_EXPERTS
    aff = jax.nn.softmax((h @ w_router).astype(jnp.float32), axis=-1)
    gates, idx = lax.top_k(jnp.swapaxes(aff, 1, 2), cap)
    xe = jax.vmap(lambda hb, ib: hb[ib])(h, idx)
    hid = jax.nn.silu(jnp.einsum('becd,edf->becf', xe, w_gate)) * jnp.einsum('becd,edf->becf', xe, w_up)
    ye = jnp.einsum('becf,efd->becd', hid, w_down) * gates[..., None].astype(h.dtype)
    return jax.vmap(lambda yb, ib: jnp.zeros((n, d), yb.dtype).at[ib.reshape(-1)].add(yb.reshape(-1, d)))(ye, idx)


def setup_inputs(seed: int = 0) -> dict:
    key = jax.random.key(seed)
    ks = jax.random.split(key, 21)
    D = D_MODEL

    def nrm(k, shape, s):
        return jax.random.normal(k, shape, jnp.float32) * s

    return {
        "x": nrm(ks[0], (BATCH, SEQ, D), 1.0),
        "c": nrm(ks[1], (BATCH, D), 1.0),
        "ctx": nrm(ks[2], (BATCH, CTX_LEN, D), 1.0),
        "c_ctx": nrm(ks[3], (D,), 1.0),
        "w_ada": nrm(ks[4], (DEPTH, D, N_MOD * D), 0.5 * D ** -0.5),
        "b_ada": nrm(ks[5], (DEPTH, N_MOD * D), 0.01),
        "norm1_g": 1.0 + nrm(ks[6], (DEPTH, D), 0.05),
        "norm2_g": 1.0 + nrm(ks[7], (DEPTH, D), 0.05),
        "w_in": nrm(ks[8], (DEPTH, D, P_IN), D ** -0.5),
        "gla_w_a_up": nrm(ks[9], (DEPTH, 2, GLA_RANK, GLA_DK), GLA_RANK ** -0.5),
        "gla_b_a": nrm(ks[10], (DEPTH, 2, GLA_DK), 0.1),
        "gla_norm_g": 1.0 + nrm(ks[11], (DEPTH, GLA_HV), 0.05),
        "gla_w_o": nrm(ks[12], (DEPTH, GLA_DV, D), GLA_DV ** -0.5),
        "conv_w": nrm(ks[13], (DEPTH, CONV_K, CONV_CH), CONV_K ** -0.5),
        "conv_w_out": nrm(ks[14], (DEPTH, CONV_CH, D), CONV_CH ** -0.5),
        "merge_w_out": nrm(ks[15], (DEPTH, D, D), D ** -0.5),
        "router_w": nrm(ks[16], (DEPTH, D, N_EXPERTS), D ** -0.5),
        "exp_w_gate": nrm(ks[17], (DEPTH, N_EXPERTS, D, D_EXPERT), D ** -0.5),
        "exp_w_up": nrm(ks[18], (DEPTH, N_EXPERTS, D, D_EXPERT), D ** -0.5),
        "exp_w_down": nrm(ks[19], (DEPTH, N_EXPERTS, D_EXPERT, D), D_EXPERT ** -0.5),
        "final_g": 1.0 + nrm(ks[20], (D,), 0.05),
    }


def reference(x, c, ctx, c_ctx, w_ada, b_ada, norm1_g, norm2_g, w_in, gla_w_a_up, gla_b_a,
              gla_norm_g, gla_w_o, conv_w, conv_w_out, merge_w_out, router_w,
              exp_w_gate, exp_w_up, exp_w_down, final_g):
    bsz, n, _ = x.shape
    rows = n // GRID_W
    c_ctx_b = jnp.broadcast_to(c_ctx, c.shape)
    for i in range(DEPTH):
        sh1, sc1, gt1, sh2, sc2, gt2 = adaln(c, w_ada[i], b_ada[i])
        csh1, csc1, cgt1, csh2, csc2, cgt2 = adaln(c_ctx_b, w_ada[i], b_ada[i])
        mixer_w = (gla_norm_g[i], gla_w_o[i], conv_w[i], conv_w_out[i], merge_w_out[i])
        hc = modulate(rms_norm(ctx, norm1_g[i]), csh1, csc1)
        pc = project(hc, w_in[i], gla_w_a_up[i], gla_b_a[i])
        if i < DEPTH - 1:
            zero_state = jnp.zeros((bsz, GLA_HEADS, GLA_HK, GLA_HV), jnp.float32)
            yc, s_f, s_b = token_mixer(pc, None, zero_state, zero_state, *mixer_w)
        else:
            s_f, s_b = context_states(pc)
        h = modulate(rms_norm(x, norm1_g[i]), sh1, sc1)
        y, _, _ = token_mixer(project(h, w_in[i], gla_w_a_up[i], gla_b_a[i]), rows, s_f, s_b, *mixer_w)
        x = x + gt1 * y
        h = modulate(rms_norm(x, norm2_g[i]), sh2, sc2)
        x = x + gt2 * expert_choice_ffn(h, router_w[i], exp_w_gate[i], exp_w_up[i], exp_w_down[i])
        if i < DEPTH - 1:
            ctx = ctx + cgt1 * yc
            hc = modulate(rms_norm(ctx, norm2_g[i]), csh2, csc2)
            ctx = ctx + cgt2 * expert_choice_ffn(hc, router_w[i], exp_w_gate[i], exp_w_up[i], exp_w_down[i])
    return rms_norm(x, final_g)
```

```python
import contextlib
import os
import numpy as np
import concourse.bass as bass
import concourse.mybir as mybir
from concourse.bass_utils import run_bass_kernel_spmd

F32 = mybir.dt.float32
BF16 = mybir.dt.bfloat16
I32 = mybir.dt.int32
ALU = mybir.AluOpType
AF = mybir.ActivationFunctionType
AX = mybir.AxisListType

NT = 2048
NCH = 16
D = 1024
CAP = 384
NROW = NT + CAP
NE = 16
KTOP = 1024
NBIS = 25
EPS = 1e-6
O_Q, O_K, O_V, O_G, O_LRF, O_LRB, O_U, O_BG, O_CG, O_RG, O_RC = (
    0, 512, 1024, 2048, 3072, 3088, 3104, 4128, 5152, 6176, 7200)
DEBUG = bool(int(os.environ.get("MK_DEBUG", "0")))


class Buf:
    def __init__(self, name):
        self.name = name
        self.w = []
        self.r = []


class Prog:
    ENGS = ("pe", "act", "dve", "pool", "sp")
    NDMA = {"sp": 14, "act": 8, "pool": 14}

    def __init__(self, nc):
        self.nc = nc
        self.stack = contextlib.ExitStack()
        self.q = {e: [] for e in self.ENGS}
        self.sems = {}
        self.cnt = {}
        self.known = {e: {} for e in self.ENGS}
        for e in self.ENGS:
            self._sem("c_" + e)
        self.dma_rr = {e: 0 for e in self.NDMA}
        for e, n in self.NDMA.items():
            for i in range(n):
                self._sem("d_%s%d" % (e, i))
        self.nbuf = 0

    def _sem(self, key):
        self.sems[key] = self.stack.enter_context(self.nc.semaphore(key))
        self.cnt[key] = 0

    def sb(self, name, shape, dtype=F32):
        return self.stack.enter_context(self.nc.sbuf_tensor("s_" + name, list(shape), dtype))

    def ps(self, name, shape, dtype=F32):
        return self.stack.enter_context(self.nc.psum_tensor("p_" + name, list(shape), dtype))

    def buf(self, name=None):
        self.nbuf += 1
        return Buf(name or "b%d" % self.nbuf)

    def emit(self, eng, fn, reads=(), writes=(), dma=False, extra_sem=None, accw=False):
        deps = {}

        def add(tok):
            if tok is None:
                return
            k, v = tok
            if deps.get(k, 0) < v:
                deps[k] = v

        for b in reads:
            for t in b.w:
                add(t)
        for b in writes:
            if not accw:
                for t in b.w:
                    add(t)
            for t in b.r:
                add(t)
        if dma:
            i = self.dma_rr[eng]
            self.dma_rr[eng] = (i + 1) % self.NDMA[eng]
            key = "d_%s%d" % (eng, i)
            add((key, self.cnt[key]))
            self.cnt[key] += 16
            tok = (key, self.cnt[key])
            inc = 16
        elif extra_sem is not None:
            key = extra_sem
            self._sem(key)
            self.cnt[key] = 1
            tok = (key, 1)
            inc = None
        else:
            key = "c_" + eng
            self.cnt[key] += 1
            tok = (key, self.cnt[key])
            inc = 1
        waits = []
        kn = self.known[eng]
        own = "c_" + eng
        for k, v in deps.items():
            if v <= 0:
                continue
            if k == own and eng == "pe":
                continue
            if kn.get(k, 0) >= v:
                continue
            kn[k] = v
            waits.append((k, v))
        self.q[eng].append((waits, fn, key, inc))
        for b in writes:
            if accw:
                b.w.append(tok)
            else:
                b.w = [tok]
            b.r = []
        for b in reads:
            b.r.append(tok)
        return tok

    def emit_wait(self, eng, toks):
        waits = []
        kn = self.known[eng]
        for k, v in toks:
            if v <= 0 or kn.get(k, 0) >= v:
                continue
            kn[k] = v
            waits.append((k, v))
        if waits:
            self.q[eng].append((waits, None, None, None))

    def barrier(self):
        toks = [(k, v) for k, v in self.cnt.items() if v > 0]
        for e in self.ENGS:
            self.emit_wait(e, toks)

    def build(self):
        nc = self.nc
        with nc.Block() as block:

            def run(name):
                def f(e):
                    for waits, fn, key, inc in self.q[name]:
                        for k, v in waits:
                            e.wait_ge(self.sems[k], v)
                        if fn is None:
                            continue
                        ins = fn(e)
                        if inc is None:
                            ins.then_inc(self.sems[key])
                        else:
                            ins.then_inc(self.sems[key], inc)
                return f

            block.tensor(run("pe"))
            block.scalar(run("act"))
            block.vector(run("dve"))
            block.gpsimd(run("pool"))
            block.sync(run("sp"))
        self.stack.close()


class Arena:
    def __init__(self, t, nwords):
        self.t = t
        self.n = nwords
        self.off = 0

    def reset(self):
        self.off = 0

    def take(self, nelem, dtype=F32):
        words = nelem if dtype in (F32, I32) else (nelem + 1) // 2
        a = self.off
        self.off += words
        assert self.off <= self.n, ("arena overflow", self.off, self.n)
        v = self.t[:, a:a + words]
        if dtype != F32:
            v = v.bitcast(dtype)
        return v


class _Stop(Exception):
    pass


def build_program(debug=False):
    nc = bass.Bass("TRN2", target_bir_lowering=False)
    P = Prog(nc)
    dbg_out = {}
    stop_at = int(os.environ.get("MK_STOP", "0"))

    def checkpoint(n):
        if stop_at == n:
            raise _Stop()

    try:
        _emit_all(nc, P, dbg_out, debug, checkpoint)
    except _Stop:
        pass
    P.emit_wait("sp", [(k_, v_) for k_, v_ in P.cnt.items() if v_ > 0])
    P.build()
    return nc, dbg_out


def _emit_all(nc, P, dbg_out, debug, checkpoint):

    def din(name, shape, dt=F32):
        return nc.dram_tensor(name, list(shape), dt, kind="ExternalInput")

    x_d = din("x", [NT, D])
    ctx_d = din("ctx", [256, D])
    cc_d = din("cc", [128, 16])
    cm_d = din("cmask", [128, 8])
    wada_d = din("w_ada", [D, 6 * D])
    bada_d = din("b_ada", [1, 6 * D])
    n1_d = din("norm1_g", [1, D])
    n2_d = din("norm2_g", [1, D])
    win_d = din("w_in", [D, 8224])
    wup_d = din("w_a_up", [32, 512])
    ba_d = din("b_a", [2, 512])
    gn_d = din("gn_col", [128, 2])
    wo_d = din("gla_w_o", [D, D])
    cw_d = din("cw_col", [128, 24])
    cwo_d = din("conv_w_out", [D, D])
    mw_d = din("merge_w_out", [D, D])
    rw_d = din("router_w", [D, NE])
    n_we = 1 if int(os.environ.get("MK_STOP", "0")) not in (0, 7, 8) else NE
    wg_d = din("exp_w_gate", [n_we, D, 2 * D])
    wu_d = din("exp_w_up", [n_we, D, 2 * D])
    wd_d = din("exp_w_down", [n_we, 2 * D, D])
    fg_d = din("final_g", [1, D])
    out_d = nc.dram_tensor("out", [NT, D], F32, kind="ExternalOutput")
    mod_d = nc.dram_tensor("mod_s", [2, 6 * D], F32)
    agin_p = [nc.dram_tensor("ag_in%d" % i, [128, 1028], F32) for i in range(2)]
    agout_p = [nc.dram_tensor("ag_out%d" % i, [512, 1028], F32) for i in range(2)]
    b_agin_p = [P.buf() for _ in range(2)]
    b_agout_p = [P.buf() for _ in range(2)]
    h2_d = nc.dram_tensor("h2_s", [NROW, D], BF16)
    acc_d = nc.dram_tensor("acc_s", [NROW, D], F32)
    affin_d = nc.dram_tensor("aff_in", [NE, NT], F32)
    affout_d = nc.dram_tensor("aff_out", [4 * NE, NT], F32)
    row_d = nc.dram_tensor("row_s", [NE, 5, CAP], F32)
    b_mod_d, b_agin_d, b_agout_d, b_h2_d, b_acc_d, b_affin_d, b_affout_d, b_out_d = [P.buf() for _ in range(8)]
    b_row_d = [P.buf() for _ in range(NE)]

    b_dbg = P.buf()

    def dbg(name, ap, shape, rd, dt=F32):
        if not debug:
            return
        t = nc.dram_tensor("dbg_" + name, list(shape), dt, kind="ExternalOutput")
        dbg_out[name] = t
        P.emit("sp", lambda e: e.dma_start(out=t.ap(), in_=ap), rd, [b_dbg], dma=True, accw=True)

    def mm(out, lhsT, rhs, st, sp_, rd, wr):
        P.emit("pe", lambda e: e.matmul(out, lhsT=lhsT, rhs=rhs, start=st, stop=sp_), rd, wr)

    def tr(out, in_, ident, rd, wr):
        P.emit("pe", lambda e: e.transpose(out=out, in_=in_, identity=ident), rd, wr)

    def act(out, in_, func, rd, wr, **kw):
        P.emit("act", lambda e: e.activation(out=out, in_=in_, func=func, **kw), rd, wr)

    def tt(eng, out, in0, in1, op, rd, wr):
        P.emit(eng, lambda e: e.tensor_tensor(out=out, in0=in0, in1=in1, op=op), rd, wr)

    def ts(eng, out, in0, s1, s2, op0, op1, rd, wr, accum=None):
        if accum is None:
            if s2 is None:
                P.emit(eng, lambda e: e.tensor_scalar(out=out, in0=in0, scalar1=s1, scalar2=None, op0=op0), rd, wr)
            else:
                P.emit(eng, lambda e: e.tensor_scalar(out=out, in0=in0, scalar1=s1, scalar2=s2, op0=op0, op1=op1), rd, wr)
        else:
            P.emit(eng, lambda e: e.tensor_scalar(out=out, in0=in0, scalar1=s1, scalar2=s2, op0=op0, op1=op1,
                                                 accum_out=accum), rd, wr)

    def stt(eng, out, in0, scalar, in1, op0, op1, rd, wr):
        eng = "dve"
        P.emit(eng, lambda e: e.scalar_tensor_tensor(out=out, in0=in0, scalar=scalar, in1=in1, op0=op0, op1=op1), rd, wr)

    def cp(eng, out, in_, rd, wr):
        P.emit(eng, lambda e: e.tensor_copy(out=out, in_=in_), rd, wr)

    def dma(eng, out, in_, rd, wr, accw=False, slow=False):
        q_ = "pool" if eng == "poolq" else "sp"
        if slow:
            P.emit(q_, lambda e: e.dma_start(out=out, in_=in_, allow_slow_non_contiguous=True), rd, wr, dma=True, accw=accw)
        else:
            P.emit(q_, lambda e: e.dma_start(out=out, in_=in_), rd, wr, dma=True, accw=accw)

    def memset(eng, ap, val, wr):
        P.emit(eng, lambda e: e.memset(ap, val), (), wr)

    def asel(out, in_, pattern, op, fill, base, cm, rd, wr):
        P.emit("pool", lambda e: e.affine_select(out=out, in_=in_, pattern=pattern, compare_op=op, fill=fill,
                                                 base=base, channel_multiplier=cm), rd, wr)

    def load_w(dst, src2d, wr, rd=()):
        P.emit("pool", lambda e: e.dma_start(out=dst, in_=src2d.rearrange("(k p) n -> p k n", p=128)), rd, wr, dma=True)

    R0f = P.sb("R0", [128, 8192], F32)
    R1 = P.sb("R1", [128, 16384], BF16)
    R2 = P.sb("R2", [128, 16384], BF16)
    R3f = P.sb("R3", [128, 8192], F32)
    SCt = P.sb("SC", [128, 9216], F32)
    SC = Arena(SCt, 9216)
    R0b = R0f[:].bitcast(BF16)
    R3b = R3f[:].bitcast(BF16)
    hT = R0b.rearrange("p (k t) -> p k t", k=8)
    b_hT = [P.buf("hT%d" % i) for i in range(NCH)]
    w4 = lambda R, i: R[:, i * 4096:(i + 1) * 4096].rearrange("p (k n) -> p k n", k=8)

    ident = P.sb("ident", [128, 128], F32); b_ident = P.buf()
    ident_b = P.sb("ident_b", [128, 128], BF16)
    TIf = P.sb("TIf", [128, 128], F32); TIb = P.sb("TIb", [128, 128], F32)
    TCf = P.sb("TCf", [128, 128], F32); TCb = P.sb("TCb", [128, 128], F32)
    SLT = P.sb("SLT", [128, 128], F32)
    Mf4 = P.sb("Mf4", [128, 512], F32); Mb4 = P.sb("Mb4", [128, 512], F32)
    ones_f = P.sb("ones_f", [128, 128], F32)
    Gm = P.sb("Gm", [128, 128], F32)
    b_const = P.buf("const")
    cst = P.sb("cst", [128, 4], F32)
    iota_s = P.sb("iota_s", [128, CAP], F32)
    basecol = P.sb("basecol", [128, 3], F32)
    pcol = P.sb("pcol", [128, 1], F32)
    wlr = P.sb("wlr", [128, 8, 32], BF16); b_wlr = P.buf()
    wupa = [P.sb("wupa%d" % i, [17, 512], F32) for i in range(2)]; b_wupa = P.buf()
    lrTa = [P.sb("lrTa%d" % i, [17, 128], F32) for i in range(2)]; b_lrTa = [P.buf(), P.buf()]
    gn = P.sb("gn", [128, 2], F32)
    cwc = P.sb("cwc", [128, 24], F32)
    cmask = P.sb("cmask", [128, 8], F32)
    bc_t = [P.sb("bc%d" % i, [128, D], F32) for i in range(3)]; b_bc = [P.buf() for _ in range(3)]
    aff = P.sb("aff", [128, NCH * NE], F32); b_aff = P.buf()
    wa2 = P.sb("wa2", [128, 8, 512], BF16); b_wa2 = P.buf()
    mrow2 = P.sb("mrow2", [2, 512], F32); brow2 = P.sb("brow2", [2, 512], F32); b_mrow2 = P.buf(); b_brow2 = P.buf()
    scc_p = P.sb("scc_p", [128, 16], BF16)
    Sinb_bf = P.sb("Sinb_bf", [128, 1024], BF16); b_Sinb_bf = P.buf()
    dB_all = P.sb("dB_all", [128, NCH * 4], F32); b_dB = P.buf()
    DcB = P.sb("DcB", [128, NCH * 4], F32); b_DcB = P.buf()

    pA = P.ps("pA", [128, 512]); pB = P.ps("pB", [128, 512]); pC = P.ps("pC", [128, 512]); pD = P.ps("pD", [128, 512])
    pV = P.ps("pV", [128, 1024]); pU = P.ps("pU", [128, 1024])
    b_pA, b_pB, b_pC, b_pD, b_pV, b_pU = [P.buf(n) for n in ("pA", "pB", "pC", "pD", "pV", "pU")]

    b_wa = [P.buf() for _ in range(4)]
    for cb_ in range(4):
        load_w(w4(R1, cb_), wada_d.ap()[:, cb_ * 512:(cb_ + 1) * 512], [b_wa[cb_]])
    cc_sb = SC.take(16, F32); scc = scc_p[:]; b_cc = P.buf()
    dma("sp", cc_sb, cc_d.ap(), (), [b_cc])
    iot_i = SC.take(128 * 0 + CAP, I32)
    tmp_i = SC.take(128, I32)
    b_tmp = P.buf()
    P.emit("pool", lambda e: e.memset(ident[:], 1.0), (), [b_ident])
    asel(ident[:], ident[:], [[-1, 128]], ALU.is_equal, 0.0, 0, 1, [b_ident], [b_ident])
    cp("dve", ident_b[:], ident[:], [b_ident], [b_ident])
    CND = {"ge": (-1, 1, ALU.is_ge), "gt": (-1, 1, ALU.is_gt), "le": (1, -1, ALU.is_ge), "lt": (1, -1, ALU.is_gt)}
    for t_, cnd, val in ((TIf, "le", -1.0 / 16), (TIb, "ge", -1.0 / 16), (TCf, "gt", -1.0 / 16),
                         (TCb, "lt", -1.0 / 16), (SLT, "lt", 1.0)):
        st_, cm_, op_ = CND[cnd]
        memset("pool", t_[:], val, [b_const])
        asel(t_[:], t_[:], [[st_, 128]], op_, 0.0, 0, cm_, [b_const], [b_const])
    for t_, cnd in ((Mf4, "le"), (Mb4, "ge")):
        st_, cm_, op_ = CND[cnd]
        memset("pool", t_[:], 1.0, [b_const])
        v = t_[:].rearrange("p (h j) -> p h j", h=4)
        asel(v, v, [[0, 4], [st_, 128]], op_, 0.0, 0, cm_, [b_const], [b_const])
    memset("pool", ones_f[:], 1.0, [b_const])
    memset("pool", cst[:, 0:1], 1.0, [b_const])
    memset("pool", cst[:, 1:2], EPS, [b_const])
    memset("pool", cst[:, 2:3], -1.0 / 16, [b_const])
    memset("pool", cst[:, 3:4], 0.0, [b_const])
    for i in range(2):
        memset("pool", lrTa[i][:], 1.0, [b_lrTa[i]])
    P.emit("pool", lambda e: e.iota(iot_i, pattern=[[1, CAP]], base=0, channel_multiplier=0), (), [b_tmp])
    cp("dve", iota_s[:], iot_i, [b_tmp], [b_const])
    P.emit("pool", lambda e: e.iota(iot_i[:, 0:3], pattern=[[1, 3]], base=NT, channel_multiplier=3), (), [b_tmp])
    cp("dve", basecol[:], iot_i[:, 0:3], [b_tmp], [b_const])
    P.emit("pool", lambda e: e.iota(iot_i[:, 0:1], pattern=[[0, 1]], base=0, channel_multiplier=1), (), [b_tmp])
    cp("dve", pcol[:], iot_i[:, 0:1], [b_tmp], [b_const])
    P.emit("pool", lambda e: e.iota(tmp_i, pattern=[[-1, 128]], base=128, channel_multiplier=1), (), [b_tmp])
    P.emit("dve", lambda e: e.tensor_single_scalar(out=tmp_i, in_=tmp_i, scalar=15, op=ALU.bitwise_and), [b_tmp], [b_tmp])
    P.emit("dve", lambda e: e.tensor_single_scalar(out=Gm[:], in_=tmp_i, scalar=0, op=ALU.is_equal), [b_tmp], [b_const])
    dma("sp", gn[:], gn_d.ap(), (), [b_const])
    dma("sp", cwc[:], cw_d.ap(), (), [b_const])
    dma("sp", cmask[:], cm_d.ap(), (), [b_const])
    for i in range(2):
        dma("sp", wupa[i][0:16, :], wup_d.ap()[i * 16:(i + 1) * 16, :], (), [b_wupa])
        dma("sp", wupa[i][16:17, :], ba_d.ap()[i:i + 1, :], (), [b_wupa])
    P.emit("pool", lambda e: e.dma_start(out=wlr[:], in_=win_d.ap()[:, O_LRF:O_LRF + 32].rearrange("(k p) n -> p k n", p=128)),
           (), [b_wlr], dma=True)
    mrow = SC.take(512, F32); brow = SC.take(512, F32); b_mrow = P.buf(); b_brow = P.buf()
    act(scc, cc_sb, AF.Silu, [b_cc], [b_cc])

    def adaln_block(cb):
        slot = w4(R1, cb % 4)
        if cb >= 4:
            load_w(slot, wada_d.ap()[:, cb * 512:(cb + 1) * 512], [b_wa[cb % 4]])
        for i in range(2):
            dma("sp", brow[i:i + 1, :], bada_d.ap()[0:1, cb * 512:(cb + 1) * 512], (), [b_brow])
        for kc in range(8):
            mm(pA[0:2, :], scc[:, kc:16:8], slot[:, kc, :], kc == 0, kc == 7, [b_cc, b_wa[cb % 4]], [b_pA])
        tt("dve", mrow[0:2, :], pA[0:2, :], brow[0:2, :], ALU.add, [b_pA, b_brow], [b_mrow])
        dma("sp", mod_d.ap()[:, cb * 512:(cb + 1) * 512], mrow[0:2, :], [b_mrow], [b_mod_d])

    for cb in range(4):
        adaln_block(cb)

    def adaln_block_late(cb):
        load_w(wa2[:], wada_d.ap()[:, cb * 512:(cb + 1) * 512], [b_wa2])
        for i in range(2):
            dma("sp", brow2[i:i + 1, :], bada_d.ap()[0:1, cb * 512:(cb + 1) * 512], (), [b_brow2])
        for kc in range(8):
            mm(pA[0:2, :], scc[:, kc:16:8], wa2[:, kc, :], kc == 0, kc == 7, [b_cc, b_wa2], [b_pA])
        tt("dve", mrow2[0:2, :], pA[0:2, :], brow2[0:2, :], ALU.add, [b_pA, b_brow2], [b_mrow2])
        dma("sp", mod_d.ap()[:, cb * 512:(cb + 1) * 512], mrow2[0:2, :], [b_mrow2], [b_mod_d])

    checkpoint(1)
    def bc_load(i, src_row):
        dma("sp", bc_t[i][:], src_row.broadcast_to([128, D]), [b_mod_d], [b_bc[i]])

    def make_AB(row, sc_off, sh_off, ng_d):
        bc_load(0, mod_d.ap()[row:row + 1, sc_off:sc_off + D])
        bc_load(2, ng_d.ap()[0:1, :])
        stt("dve", bc_t[0][:], bc_t[0][:], 1.0, bc_t[2][:], ALU.add, ALU.mult, [b_bc[0], b_bc[2]], [b_bc[0]])
        bc_load(1, mod_d.ap()[row:row + 1, sh_off:sh_off + D])

    ND1 = 3
    xt = [SC.take(D, F32) for _ in range(ND1)]; b_xt = [P.buf() for _ in range(ND1)]
    hf = [SC.take(D, F32) for _ in range(ND1)]; b_hf = [P.buf() for _ in range(ND1)]
    junk_ = [SC.take(D, BF16) for _ in range(2)]; b_junk_ = [P.buf(), P.buf()]
    stat_ = [SC.take(8, F32) for _ in range(ND1)]; b_stat_ = [P.buf() for _ in range(ND1)]

    def norm_tile(src, b_src, i, A, b_A, B, b_B):
        junk, b_junk, stat, b_stat = junk_[i % 2], b_junk_[i % 2], stat_[i], b_stat_[i]
        act(junk, src, AF.Square, [b_src], [b_junk, b_stat], accum_out=stat[:, 0:1])
        act(stat[:, 1:2], stat[:, 0:1], AF.Ln, [b_stat, b_const], [b_stat], scale=1.0 / D, bias=cst[:, 1:2])
        act(stat[:, 2:3], stat[:, 1:2], AF.Exp, [b_stat], [b_stat], scale=-0.5)
        stt("dve", hf[i], src, stat[:, 2:3], A, ALU.mult, ALU.mult, [b_src, b_stat, b_A], [b_hf[i]])
        if B is not None:
            tt("dve", hf[i], hf[i], B, ALU.add, [b_hf[i], b_B], [b_hf[i]])

    tcount = [0]

    def transpose_to(i, dst3, b_dst, t0):
        pX, b_pX = (pV, b_pV) if tcount[0] % 2 == 0 else (pU, b_pU)
        tcount[0] += 1
        for kc in range(8):
            tr(pX[:, kc * 128:(kc + 1) * 128], hf[i][:, kc * 128:(kc + 1) * 128], ident[:], [b_hf[i], b_ident], [b_pX])
        act(dst3[:, :, t0:t0 + 128], pX[:].rearrange("p (k t) -> p k t", k=8), AF.Copy, [b_pX], [b_dst])

    wq, wk, wv0, wv1 = [w4(R2, i) for i in range(4)]
    b_wq, b_wk, b_wv = P.buf(), P.buf(), P.buf()
    load_w(wk, win_d.ap()[:, O_K:O_K + 512], [b_wk])
    load_w(wv0, win_d.ap()[:, O_V:O_V + 512], [b_wv])
    load_w(wv1, win_d.ap()[:, O_V + 512:O_V + 1024], [b_wv])
    load_w(wq, win_d.ap()[:, O_Q:O_Q + 512], [b_wq])

    make_AB(1, 1 * D, 0 * D, n1_d)
    cT = R3b[:, 8192:8192 + 2048].rearrange("p (k t) -> p k t", k=8)
    b_cT = P.buf()
    for i in range(2):
        dma("poolq", xt[i], ctx_d.ap()[i * 128:(i + 1) * 128, :], (), [b_xt[i]])
        norm_tile(xt[i], b_xt[i], i, bc_t[0][:], b_bc[0], bc_t[1][:], b_bc[1])
        transpose_to(i, cT, b_cT, i * 128)
    make_AB(0, 1 * D, 0 * D, n1_d)

    def n1_stage1(c_):
        i_ = c_ % ND1
        dma("poolq", xt[i_], x_d.ap()[c_ * 128:(c_ + 1) * 128, :], (), [b_xt[i_]])
        norm_tile(xt[i_], b_xt[i_], i_, bc_t[0][:], b_bc[0], bc_t[1][:], b_bc[1])

    n1_stage1(0)
    for c in range(NCH):
        if c + 1 < NCH:
            n1_stage1(c + 1)
        transpose_to(c % ND1, hT, b_hT[c], c * 128)
    if debug:
        hdb = hf[1]; b_hdb = b_hf[1]
        cp("dve", hdb, hT[:, :, 0:128], [b_hT[0]], [b_hdb])
        dbg("hT0", hdb, [128, D], [b_hdb])
    checkpoint(2)
    P.barrier()
    zt = bc_t[1][:]
    b_zt = b_bc[1]
    memset("dve", zt, 0.0, [b_zt])
    for i in range(NROW // 128):
        dma("sp", acc_d.ap()[i * 128:(i + 1) * 128, :], zt, [b_zt], [b_acc_d], accw=True)
    ztb = zt.bitcast(BF16)[:, 0:D]
    for i in range(CAP // 128):
        dma("sp", h2_d.ap()[NT + i * 128:NT + (i + 1) * 128, :], ztb, [b_zt], [b_h2_d], accw=True)

    SC.reset()
    l_sb = [SC.take(512, F32) for _ in range(2)]; b_l = [P.buf(), P.buf()]
    e_tmp = SC.take(512, F32); b_etmp = P.buf()
    v_bf = SC.take(1024, BF16); b_v = P.buf()
    edec = SC.take(512, F32); b_edec = P.buf()
    kdec = [SC.take(512, BF16) for _ in range(2)]; b_kdec = [P.buf(), P.buf()]
    sm = SC.take(32, F32); b_sm = P.buf()
    S_f = SC.take(1024, F32); b_Sf = P.buf()
    sc_mark = SC.off
    S_b = [SC.take(1024, F32) for _ in range(3)]; b_Sb = [P.buf() for _ in range(3)]
    SC.off = sc_mark
    eb = SC.take(512, F32); enb = SC.take(512, F32); b_eb = P.buf(); b_enb = P.buf()
    qin = [SC.take(512, BF16) for _ in range(2)]; kin = [SC.take(512, BF16) for _ in range(2)]
    b_qin = [P.buf(), P.buf()]; b_kin = [P.buf(), P.buf()]
    scm = [SC.take(512, BF16) for _ in range(2)]; b_scm = [P.buf(), P.buf()]
    Sf_bf = SC.take(1024, BF16); b_Sfbf = P.buf()
    on = SC.take(1024, F32); b_on = P.buf()
    qinD = SC.take(512, BF16); b_qinD = P.buf()
    Gst = R3f[:, 0:1032]; b_G = P.buf()
    Gst2 = [R3f[:, 0:1028], R3f[:, 1032:2060]]; b_G2 = [b_G, P.buf()]
    tmpS = R3f[:, 2064:3088]; b_tmpS = P.buf()
    sctx = [R3f[:, 6144:7168], R3f[:, 7168:8192]]; b_sctx = [P.buf(), P.buf()]
    R1v = R1[:].rearrange("p (c n) -> p c n", c=NCH)
    b_R1 = [P.buf("R1_%d" % c) for c in range(NCH)]
    d_f = sm[:, 0:4]; d_b = sm[:, 4:8]

    def chunk_kvl(src3, b_src, t0, need_b):
        for kc in range(8):
            mm(pC[:], src3[:, kc, t0:t0 + 128], wk[:, kc, :], kc == 0, kc == 7, [b_src, b_wk], [b_pC])
        for hv, wv in enumerate((wv0, wv1)):
            for kc in range(8):
                mm(pV[:, hv * 512:(hv + 1) * 512], src3[:, kc, t0:t0 + 128], wv[:, kc, :], kc == 0, kc == 7,
                   [b_src, b_wv], [b_pV])
        for i in range(2):
            for kc in range(8):
                mm(pD[0:16, i * 128:(i + 1) * 128], wlr[:, kc, i * 16:(i + 1) * 16], src3[:, kc, t0:t0 + 128],
                   kc == 0, kc == 7, [b_src, b_wlr], [b_pD])
        act(v_bf, pV[:], AF.Copy, [b_pV], [b_v])
        for i in range(2):
            cp("dve", lrTa[i][0:16, :], pD[0:16, i * 128:(i + 1) * 128], [b_pD], [b_lrTa[i]])
        for i in range(2):
            mm(pD[:], lrTa[i][:], wupa[i][:], True, True, [b_lrTa[i], b_wupa], [b_pD])
            act(e_tmp, pD[:], AF.Exp, [b_pD], [b_etmp], scale=-1.0)
            act(l_sb[i], e_tmp, AF.Ln, [b_etmp, b_const], [b_l[i]], bias=cst[:, 0:1])
        dirs = (0, 1) if need_b else (0,)
        for i in dirs:
            TC = TCf if i == 0 else TCb
            mm(pD[:], TC[:], l_sb[i], True, True, [b_const, b_l[i]], [b_pD])
            act(edec, pD[:], AF.Exp, [b_pD], [b_edec])
            tt("dve", kdec[i], pC[:], edec, ALU.mult, [b_pC, b_edec], [b_kdec[i]])
            for h in range(4):
                mm(pU[:, i * 4 + h:i * 4 + h + 1], l_sb[i][:, h * 128:(h + 1) * 128], cst[:, 2:3], True, True,
                   [b_l[i], b_const], [b_pU])
        nd = 8 if need_b else 4
        act(sm[:, 0:nd], pU[:, 0:nd], AF.Exp, [b_pU], [b_sm])

    def u_mm(i):
        for h in range(4):
            mm(pU[:, h * 256:(h + 1) * 256], kdec[i][:, h * 128:(h + 1) * 128], v_bf[:, h * 256:(h + 1) * 256], True, True,
               [b_kdec[i], b_v], [b_pU])

    def state_update(S, b_S, dcol, Sout=None, b_Sout=None):
        Sout = S if Sout is None else Sout
        b_Sout = b_S if b_Sout is None else b_Sout
        for h in range(4):
            stt("dve", Sout[:, h * 256:(h + 1) * 256], S[:, h * 256:(h + 1) * 256], dcol[:, h:h + 1],
                pU[:, h * 256:(h + 1) * 256], ALU.mult, ALU.add, [b_S, b_sm, b_pU], [b_Sout])

    ub0 = Gst[:, 0:1024]
    memset("dve", sctx[0], 0.0, [b_sctx[0]])
    for c in range(2):
        chunk_kvl(cT, b_cT, c * 128, True)
        u_mm(0)
        state_update(sctx[0], b_sctx[0], d_f)
        u_mm(1)
        if c == 0:
            cp("dve", ub0, pU[:], [b_pU], [b_G])
            cp("dve", sm[:, 24:28], d_b, [b_sm], [b_sm])
        else:
            cp("dve", sctx[1], pU[:], [b_pU], [b_sctx[1]])
    for h in range(4):
        stt("dve", sctx[1][:, h * 256:(h + 1) * 256], sctx[1][:, h * 256:(h + 1) * 256], sm[:, 24 + h:25 + h],
            ub0[:, h * 256:(h + 1) * 256], ALU.mult, ALU.add, [b_sctx[1], b_sm, b_G], [b_sctx[1]])
    dbg("sctx_f", sctx[0], [128, 1024], [b_sctx[0]])
    dbg("sctx_b", sctx[1], [128, 1024], [b_sctx[1]])

    checkpoint(3)
    memset("dve", S_f, 0.0, [b_Sf])
    memset("dve", sm[:, 16:20], 1.0, [b_sm])
    for c in range(NCH):
        chunk_kvl(hT, b_hT[c], c * 128, True)
        u_mm(0)
        state_update(S_f, b_Sf, d_f)
        tt("dve", sm[:, 16:20], sm[:, 16:20], d_f, ALU.mult, [b_sm], [b_sm])
        u_mm(1)
        act(R1v[:, c, :], pU[:], AF.Copy, [b_pU], [b_R1[c]])
        cp("dve", dB_all[:, c * 4:(c + 1) * 4], d_b, [b_sm], [b_dB])
        if c % 2 == 1:
            adaln_block_late(4 + c // 2)
    PW = 1028

    def state_allgather(dr, Lsrc, b_Lsrc):
        dcol = 16 if dr == 0 else 20
        dma("sp", agin_p[dr].ap()[:, 0:1024], Lsrc, [b_Lsrc], [b_agin_p[dr]])
        dma("sp", agin_p[dr].ap()[:, 1024:1028], sm[:, dcol:dcol + 4], [b_sm], [b_agin_p[dr]], accw=True)
        P.emit("pool", (lambda pc_: (lambda e: e.collective_compute(
            "AllGather", ALU.bypass, replica_groups=[[0, 1, 2, 3], [4, 5, 6, 7]],
            ins=[agin_p[pc_].ap().opt()], outs=[agout_p[pc_].ap().opt()])))(dr),
            [b_agin_p[dr]], [b_agout_p[dr]], extra_sem="cc_state%d" % dr)

    if int(os.environ.get("MK_STOP", "0")) != 31:
        state_allgather(0, S_f, b_Sf)
    memset("dve", S_b[0], 0.0, [b_Sb[0]])
    memset("dve", sm[:, 20:24], 1.0, [b_sm])
    cur = 0
    for c in range(NCH - 1, -1, -1):
        nxt = (cur + 1) % 3
        for h in range(4):
            stt("dve", S_b[nxt][:, h * 256:(h + 1) * 256], S_b[cur][:, h * 256:(h + 1) * 256],
                dB_all[:, c * 4 + h:c * 4 + h + 1], R1v[:, c, h * 256:(h + 1) * 256], ALU.mult, ALU.add,
                [b_Sb[cur], b_dB, b_R1[c]], [b_Sb[nxt]])
        act(R1v[:, c, :], S_b[cur], AF.Copy, [b_Sb[cur]], [b_R1[c]])
        cp("dve", DcB[:, c * 4:(c + 1) * 4], sm[:, 20:24], [b_sm], [b_DcB])
        tt("dve", sm[:, 20:24], sm[:, 20:24], dB_all[:, c * 4:(c + 1) * 4], ALU.mult, [b_sm, b_dB], [b_sm])
        cur = nxt
    L_b = S_b[cur]; b_Lb = b_Sb[cur]
    checkpoint(31)
    state_allgather(1, L_b, b_Lb)
    checkpoint(32)
    Sinb = S_b[(cur + 1) % 3]; b_Sinb = b_Sb[(cur + 1) % 3]
    cp("dve", S_f, sctx[0], [b_sctx[0]], [b_Sf])
    cp("dve", Sinb, sctx[1], [b_sctx[1]], [b_Sinb])
    gi = 0
    for (S, b_S, order, moff, dr) in ((S_f, b_Sf, (0, 1, 2, 3), 0, 0), (Sinb, b_Sinb, (3, 2, 1, 0), 4, 1)):
        for r in order:
            G_ = Gst2[gi % 2]; b_G_ = b_G2[gi % 2]; gi += 1
            dma("sp", G_, agout_p[dr].ap()[r * 128:(r + 1) * 128, :], [b_agout_p[dr]], [b_G_])
            mcol = cmask[:, moff + r:moff + r + 1]
            ts("dve", sm[:, 24:28], G_[:, 1024:1028], -1.0, mcol, ALU.add, ALU.mult, [b_G_, b_const], [b_sm])
            ts("dve", sm[:, 24:28], sm[:, 24:28], 1.0, None, ALU.add, None, [b_sm], [b_sm])
            for h in range(4):
                ts("dve", tmpS[:, h * 256:(h + 1) * 256], S[:, h * 256:(h + 1) * 256], sm[:, 24 + h:25 + h], None,
                   ALU.mult, None, [b_S, b_sm], [b_tmpS])
                stt("dve", S[:, h * 256:(h + 1) * 256], G_[:, h * 256:(h + 1) * 256], mcol, tmpS[:, h * 256:(h + 1) * 256],
                    ALU.mult, ALU.add, [b_G_, b_const, b_tmpS], [b_S])
    dbg("Sin_f", S_f, [128, 1024], [b_Sf])
    dbg("Sin_b", Sinb, [128, 1024], [b_Sinb])
    act(Sinb_bf[:], Sinb, AF.Copy, [b_Sinb], [b_Sinb_bf])
    P.barrier()
    cp("dve", Sf_bf, S_f, [b_Sf], [b_Sfbf])

    checkpoint(4)
    ogT = R3b[:, 0:16384].rearrange("p (k t) -> p k t", k=8)
    b_og = [P.buf("og%d" % c) for c in range(NCH)]
    def projQK(c_):
        t0_ = c_ * 128
        for (pX, b_pX, w, b_w) in ((pA, b_pA, wq, b_wq), (pB, b_pB, wk, b_wk)):
            for h in range(4):
                for kc in range(8):
                    mm(pX[:, h * 128:(h + 1) * 128], w[:, kc, h * 128:(h + 1) * 128], hT[:, kc, t0_:t0_ + 128], kc == 0, kc == 7,
                       [b_w, b_hT[c_]], [b_pX])

    projQK(0)
    for c in range(NCH):
        t0 = c * 128
        chunk_kvl(hT, b_hT[c], t0, False)
        for i in range(2):
            TI = TIf if i == 0 else TIb
            for h in range(4):
                mm(pD[:, h * 128:(h + 1) * 128], l_sb[i][:, h * 128:(h + 1) * 128], TI[:], True, True, [b_l[i], b_const], [b_pD])
            act(eb, pD[:], AF.Exp, [b_pD], [b_eb])
            act(enb, pD[:], AF.Exp, [b_pD], [b_enb], scale=-1.0)
            stt("dve", qin[i], pA[:], 128.0 ** -0.5, eb, ALU.mult, ALU.mult, [b_pA, b_eb], [b_qin[i]])
            tt("dve", kin[i], pB[:], enb, ALU.mult, [b_pB, b_enb], [b_kin[i]])
        for i, (pX, b_pX, M4) in enumerate(((pA, b_pA, Mf4), (pB, b_pB, Mb4))):
            for h in range(4):
                mm(pX[:, h * 128:(h + 1) * 128], kin[i][:, h * 128:(h + 1) * 128], qin[i][:, h * 128:(h + 1) * 128], True, True,
                   [b_kin[i], b_qin[i]], [b_pX])
            tt("dve", scm[i], pX[:], M4[:], ALU.mult, [b_pX, b_const], [b_scm[i]])
        tt("dve", qinD.rearrange("p (h t) -> p h t", h=4), qin[1].rearrange("p (h t) -> p h t", h=4),
           DcB[:, c * 4:(c + 1) * 4].unsqueeze(2).broadcast_to([128, 4, 128]), ALU.mult, [b_qin[1], b_DcB], [b_qinD])
        for h in range(4):
            o_ = pV[:, h * 256:(h + 1) * 256]
            vs = v_bf[:, h * 256:(h + 1) * 256]
            mm(o_, scm[0][:, h * 128:(h + 1) * 128], vs, True, False, [b_scm[0], b_v], [b_pV])
            mm(o_, scm[1][:, h * 128:(h + 1) * 128], vs, False, False, [b_scm[1], b_v], [b_pV])
            mm(o_, qin[0][:, h * 128:(h + 1) * 128], Sf_bf[:, h * 256:(h + 1) * 256], False, False, [b_qin[0], b_Sfbf], [b_pV])
            mm(o_, qin[1][:, h * 128:(h + 1) * 128], R1v[:, c, h * 256:(h + 1) * 256], False, False, [b_qin[1], b_R1[c]], [b_pV])
            mm(o_, qinD[:, h * 128:(h + 1) * 128], Sinb_bf[:, h * 256:(h + 1) * 256], False, True, [b_qinD, b_Sinb_bf], [b_pV])
        u_mm(0)
        state_update(S_f, b_Sf, d_f)
        act(Sf_bf, S_f, AF.Copy, [b_Sf], [b_Sfbf])
        for h in range(4):
            act(e_tmp[:, 0:256], pV[:, h * 256:(h + 1) * 256], AF.Square, [b_pV], [b_etmp, b_sm],
                accum_out=sm[:, 8 + h:9 + h])
        act(sm[:, 12:16], sm[:, 8:12], AF.Ln, [b_sm, b_const], [b_sm], scale=1.0 / 256, bias=cst[:, 1:2])
        act(sm[:, 12:16], sm[:, 12:16], AF.Exp, [b_sm], [b_sm], scale=-0.5)
        for h in range(4):
            ts("dve", on[:, h * 256:(h + 1) * 256], pV[:, h * 256:(h + 1) * 256], sm[:, 12 + h:13 + h], None, ALU.mult, None,
               [b_pV, b_sm], [b_on])
        if debug and c in (0, 15):
            dbg("on%d" % c, on, [128, 1024], [b_on])
        if c + 1 < NCH:
            projQK(c + 1)
        for j in range(8):
            tr(pU[:, j * 128:(j + 1) * 128], on[:, j * 128:(j + 1) * 128], ident[:], [b_on, b_ident], [b_pU])
        act(ogT[:, :, t0:t0 + 128], pU[:].rearrange("p (k t) -> p k t", k=8), AF.Copy, [b_pU], [b_og[c]])
    P.barrier()

    checkpoint(5)
    SC.reset()
    slots = [w4(R2, i) for i in range(4)]
    b_slot = [P.buf("slot%d" % i) for i in range(4)]
    sg = [SC.take(512, F32) for _ in range(2)]; b_sg = [P.buf(), P.buf()]
    t1 = [SC.take(512, F32) for _ in range(2)]; b_t1 = [P.buf(), P.buf()]
    t2 = [SC.take(512, F32) for _ in range(2)]; b_t2 = [P.buf(), P.buf()]
    xr = [SC.take(D, F32) for _ in range(2)]; b_xr = [P.buf(), P.buf()]
    psA = [(pA, b_pA), (pB, b_pB)]
    b_ogk = [[P.buf() for _ in range(4)] for _ in range(8)]
    cntr = [0]
    slot_rr = [0]

    def next_slot():
        i_ = slot_rr[0] % 4
        slot_rr[0] += 1
        return slots[i_], b_slot[i_]

    def proj_fm(pX, b_pX, wslot, b_w, col0, src3, b_src_list, tg):
        for kc in range(8):
            mm(pX[:], wslot[:, kc, col0:col0 + 128], src3[:, kc, tg * 512:(tg + 1) * 512], kc == 0, kc == 7,
               [b_w] + b_src_list, [b_pX])

    def hT_bufs(tg):
        return [b_hT[tg * 4 + q] for q in range(4)]

    def og_bufs(tg):
        return [b_og[tg * 4 + q] for q in range(4)]

    sG = [next_slot(), next_slot()]
    for blk in range(2):
        load_w(sG[blk][0], win_d.ap()[:, O_G + blk * 512:O_G + (blk + 1) * 512], [sG[blk][1]])
    for dvc in range(8):
        for tg in range(4):
            k = cntr[0] % 2; cntr[0] += 1
            pX, b_pX = psA[k]
            proj_fm(pX, b_pX, sG[dvc // 4][0], sG[dvc // 4][1], (dvc % 4) * 128, hT, hT_bufs(tg), tg)
            act(sg[k], pX[:], AF.Silu, [b_pX], [b_sg[k]])
            stt("dve", ogT[:, dvc, tg * 512:(tg + 1) * 512], ogT[:, dvc, tg * 512:(tg + 1) * 512], gn[:, dvc % 2:dvc % 2 + 1], sg[k],
                ALU.mult, ALU.mult, og_bufs(tg) + [b_sg[k], b_const], [b_ogk[dvc][tg]] + og_bufs(tg))
    if debug:
        ogdb = SC.take(D, F32); b_ogdb = P.buf()
        cp("dve", ogdb, ogT[:, :, 0:128], og_bufs(0), [b_ogdb])
        dbg("ogT0", ogdb, [128, D], [b_ogdb])
    mT = R1[:].rearrange("p (k t) -> p k t", k=8)
    b_mT = [[P.buf() for _ in range(4)] for _ in range(8)]
    def t1_loads(grp):
        sa, sb_ = next_slot(), next_slot()
        load_w(sa[0], wo_d.ap()[:, grp * 512:(grp + 1) * 512], [sa[1]])
        load_w(sb_[0], win_d.ap()[:, O_RG + grp * 512:O_RG + (grp + 1) * 512], [sb_[1]])
        return sa, sb_

    pre = t1_loads(0)
    P.barrier()
    for grp in range(2):
        sa, sb_ = pre if grp == 0 else t1_loads(1)
        for dq in range(4):
            dc = grp * 4 + dq
            for tg in range(4):
                proj_fm(pA, b_pA, sa[0], sa[1], dq * 128, ogT, og_bufs(tg), tg)
                proj_fm(pB, b_pB, sb_[0], sb_[1], dq * 128, hT, hT_bufs(tg), tg)
                k = cntr[0] % 2; cntr[0] += 1
                act(sg[k], pB[:], AF.Sigmoid, [b_pB], [b_sg[k]])
                tt("dve", mT[:, dc, tg * 512:(tg + 1) * 512], pA[:], sg[k], ALU.mult, [b_pA, b_sg[k]], [b_mT[dc][tg]])
    def conv_loads(grp):
        s3_ = [next_slot(), next_slot(), next_slot()]
        for i, off in enumerate((O_U, O_CG, O_BG)):
            load_w(s3_[i][0], win_d.ap()[:, off + grp * 512:off + (grp + 1) * 512], [s3_[i][1]])
        return s3_

    pre3 = conv_loads(0)
    P.barrier()
    bcT = R3b[:, 0:16384].rearrange("p (k t) -> p k t", k=8)
    b_bcT = [P.buf() for _ in range(4)]
    for grp in range(2):
        s3 = pre3 if grp == 0 else conv_loads(1)
        for cq in range(4):
            chc = grp * 4 + cq
            for tg in range(4):
                k = cntr[0] % 2; cntr[0] += 1
                proj_fm(pA, b_pA, s3[0][0], s3[0][1], cq * 128, hT, hT_bufs(tg), tg)
                proj_fm(pB, b_pB, s3[1][0], s3[1][1], cq * 128, hT, hT_bufs(tg), tg)
                proj_fm(pC, b_pC, s3[2][0], s3[2][1], cq * 128, hT, hT_bufs(tg), tg)
                act(sg[k], pA[:], AF.Copy, [b_pA], [b_sg[k]])
                tt("dve", t1[k], sg[k], pB[:], ALU.mult, [b_sg[k], b_pB], [b_t1[k]])
                act(t2[k], t1[k], AF.Copy, [b_t1[k], b_const], [b_t2[k]], scale=cwc[:, 8 + chc:9 + chc])
                cu3 = t1[k].rearrange("p (r w) -> p r w", w=64)
                ac3 = t2[k].rearrange("p (r w) -> p r w", w=64)
                stt("pool", ac3[:, :, 1:64], cu3[:, :, 0:63], cwc[:, chc:chc + 1], ac3[:, :, 1:64], ALU.mult, ALU.add,
                    [b_t1[k], b_t2[k], b_const], [b_t2[k]])
                stt("pool", ac3[:, :, 0:63], cu3[:, :, 1:64], cwc[:, 16 + chc:17 + chc], ac3[:, :, 0:63], ALU.mult, ALU.add,
                    [b_t1[k], b_t2[k], b_const], [b_t2[k]])
                tt("dve", bcT[:, chc, tg * 512:(tg + 1) * 512], t2[k], pC[:], ALU.mult, [b_t2[k], b_pC], [b_bcT[tg]])
    if debug:
        bcdb = SC.take(D, F32); b_bcdb = P.buf()
        cp("dve", bcdb, bcT[:, :, 0:128], [b_bcT[0]], [b_bcdb])
        dbg("bcT0", bcdb, [128, D], [b_bcdb])
    for grp in range(2):
        sa, sb_ = next_slot(), next_slot()
        load_w(sa[0], cwo_d.ap()[:, grp * 512:(grp + 1) * 512], [sa[1]])
        load_w(sb_[0], win_d.ap()[:, O_RC + grp * 512:O_RC + (grp + 1) * 512], [sb_[1]])
        for dq in range(4):
            dc = grp * 4 + dq
            for tg in range(4):
                proj_fm(pA, b_pA, sa[0], sa[1], dq * 128, bcT, [b_bcT[tg]], tg)
                proj_fm(pB, b_pB, sb_[0], sb_[1], dq * 128, hT, hT_bufs(tg), tg)
                k = cntr[0] % 2; cntr[0] += 1
                act(sg[k], pB[:], AF.Sigmoid, [b_pB], [b_sg[k]])
                tt("dve", t1[k], pA[:], sg[k], ALU.mult, [b_pA, b_sg[k]], [b_t1[k]])
                tt("dve", mT[:, dc, tg * 512:(tg + 1) * 512], t1[k], mT[:, dc, tg * 512:(tg + 1) * 512], ALU.add,
                   [b_t1[k], b_mT[dc][tg]], [b_mT[dc][tg]])
    sM = [next_slot(), next_slot()]
    for blk in range(2):
        load_w(sM[blk][0], mw_d.ap()[:, blk * 512:(blk + 1) * 512], [sM[blk][1]])
    bc_load(2, mod_d.ap()[0:1, 2 * D:3 * D])
    P.barrier()

    def x1_tile(tt_):
        return R0f[:, tt_ * D:(tt_ + 1) * D] if tt_ < 8 else R3f[:, (tt_ - 8) * D:(tt_ - 7) * D]

    b_x1 = [P.buf("x1_%d" % i) for i in range(NCH)]
    SC.reset()
    xr = [SC.take(D, F32) for _ in range(2)]; b_xr = [P.buf(), P.buf()]
    ND2 = 2
    hf2 = [SC.take(D, F32) for _ in range(ND2)]; b_hf2 = [P.buf() for _ in range(ND2)]
    hb2 = [SC.take(D, BF16) for _ in range(2)]; b_hb2 = [P.buf(), P.buf()]
    h2T = [SC.take(D, F32) for _ in range(ND2)]; b_h2T = [P.buf() for _ in range(ND2)]
    junk2 = SC.take(D, BF16); b_junk2 = P.buf()
    wr_sb = SC.take(8 * NE, F32); b_wr = P.buf()
    st2_ = [SC.take(16, F32) for _ in range(ND2)]; b_st2_ = [P.buf() for _ in range(ND2)]
    lg_ = [SC.take(NE, F32) for _ in range(2)]; b_lg_ = [P.buf(), P.buf()]
    affTp = [SC.take(512, F32) for _ in range(2)]; b_affTp = [P.buf(), P.buf()]
    bc_load(0, mod_d.ap()[0:1, 4 * D:5 * D])
    dma("sp", hf2[0], n2_d.ap()[0:1, :].broadcast_to([128, D]), (), [b_hf2[0]])
    stt("dve", bc_t[0][:], bc_t[0][:], 1.0, hf2[0], ALU.add, ALU.mult, [b_bc[0], b_hf2[0]], [b_bc[0]])
    bc_load(1, mod_d.ap()[0:1, 3 * D:4 * D])
    dma("sp", wr_sb.rearrange("p (k n) -> p k n", k=8), rw_d.ap().rearrange("(k p) n -> p k n", p=128), (), [b_wr])
    aff3 = aff[:].rearrange("p (t e) -> p t e", e=NE)
    pLg = [(pA, b_pA), (pC, b_pC)]
    def y_tile(c):
        k = c % 2
        dma("poolq", xr[k], x_d.ap()[c * 128:(c + 1) * 128, :], (), [b_xr[k]])
        for blk in range(2):
            for kc in range(8):
                mm(pV[:, blk * 512:(blk + 1) * 512], mT[:, kc, c * 128:(c + 1) * 128], sM[blk][0][:, kc, :], kc == 0, kc == 7,
                   [b_mT[kc][c // 4], sM[blk][1]], [b_pV])
        tt("dve", x1_tile(c), pV[:], bc_t[2][:], ALU.mult, [b_pV, b_bc[2]], [b_x1[c]])
        tt("dve", x1_tile(c), x1_tile(c), xr[k], ALU.add, [b_x1[c], b_xr[k]], [b_x1[c]])

    def n2_stage1(c):
        i = c % ND2
        i2 = c % 2
        src = x1_tile(c)
        st2, b_st2 = st2_[i], b_st2_[i]
        act(junk2, src, AF.Square, [b_x1[c]], [b_junk2, b_st2], accum_out=st2[:, 0:1])
        act(st2[:, 1:2], st2[:, 0:1], AF.Ln, [b_st2, b_const], [b_st2], scale=1.0 / D, bias=cst[:, 1:2])
        act(st2[:, 2:3], st2[:, 1:2], AF.Exp, [b_st2], [b_st2], scale=-0.5)
        stt("dve", hf2[i], src, st2[:, 2:3], bc_t[0][:], ALU.mult, ALU.mult, [b_x1[c], b_st2, b_bc[0]], [b_hf2[i]])
        tt("dve", hf2[i], hf2[i], bc_t[1][:], ALU.add, [b_hf2[i], b_bc[1]], [b_hf2[i]])
        act(hb2[i2], hf2[i], AF.Copy, [b_hf2[i]], [b_hb2[i2]])
        dma("sp", h2_d.ap()[c * 128:(c + 1) * 128, :], hb2[i2], [b_hb2[i2]], [b_h2_d], accw=True)
        for kc in range(8):
            tr(pU[:, kc * 128:(kc + 1) * 128], hf2[i][:, kc * 128:(kc + 1) * 128], ident[:], [b_hf2[i], b_ident], [b_pU])
        act(h2T[i], pU[:], AF.Copy, [b_pU], [b_h2T[i]])

    def n2_stage2(c):
        i = c % ND2
        i2 = c % 2
        st2, b_st2, lg, b_lg = st2_[i], b_st2_[i], lg_[i2], b_lg_[i2]
        pL, b_pL = pLg[i2]
        for kc in range(8):
            mm(pL[:, 0:NE], h2T[i][:, kc * 128:(kc + 1) * 128], wr_sb[:, kc * NE:(kc + 1) * NE], kc == 0, kc == 7,
               [b_h2T[i], b_wr], [b_pL])
        P.emit("dve", (lambda st_, pL_: (lambda e: e.tensor_reduce(out=st_[:, 4:5], in_=pL_[:, 0:NE], axis=AX.X, op=ALU.max)))(st2, pL),
               [b_pL], [b_st2])
        ts("dve", st2[:, 5:6], st2[:, 4:5], -1.0, None, ALU.mult, None, [b_st2], [b_st2])
        act(lg, pL[:, 0:NE], AF.Exp, [b_pL, b_st2], [b_lg, b_st2], bias=st2[:, 5:6], accum_out=st2[:, 6:7])
        P.emit("dve", (lambda st_: (lambda e: e.reciprocal(out=st_[:, 7:8], in_=st_[:, 6:7])))(st2), [b_st2], [b_st2])
        ts("dve", aff3[:, c, :], lg, st2[:, 7:8], None, ALU.mult, None, [b_lg, b_st2], [b_aff])
        tr(pB[0:NE, (c % 4) * 128:(c % 4 + 1) * 128], aff3[:, c, :], ident[:], [b_aff, b_ident], [b_pB])
        if c % 4 == 3:
            g_ = c // 4
            cp("dve", affTp[g_ % 2][0:NE, :], pB[0:NE, :], [b_pB], [b_affTp[g_ % 2]])
            dma("sp", affin_d.ap()[:, g_ * 512:(g_ + 1) * 512], affTp[g_ % 2][0:NE, :], [b_affTp[g_ % 2]], [b_affin_d], accw=True)

    y_tile(0)
    y_tile(1)
    n2_stage1(0)
    for c in range(NCH):
        if c + 2 < NCH:
            y_tile(c + 2)
        if c + 1 < NCH:
            n2_stage1(c + 1)
        n2_stage2(c)
    dbg("x1_0", x1_tile(0), [128, D], [b_x1[0]])
    dbg("x1_15", x1_tile(15), [128, D], [b_x1[15]])
    P.barrier()

    wd_sb = R1[:].rearrange("p (k n) -> p k n", k=16)
    b_wd = P.buf()
    gu = [[w4(R2, 0), w4(R2, 1)], [w4(R2, 2), w4(R2, 3)]]
    b_gu = [[P.buf(), P.buf()], [P.buf(), P.buf()]]

    def load_A(e, fb):
        s_ = fb % 2
        load_w(gu[s_][0], wg_d.ap()[e][:, fb * 512:(fb + 1) * 512], [b_gu[s_][0]])
        load_w(gu[s_][1], wu_d.ap()[e][:, fb * 512:(fb + 1) * 512], [b_gu[s_][1]])

    def load_wd(e):
        P.emit("pool", (lambda e__: (lambda e_: e_.dma_start(
            out=wd_sb, in_=wd_d.ap()[e__].rearrange("(k p) n -> p k n", p=128))))(e), (), [b_wd], dma=True)

    if int(os.environ.get("MK_STOP", "0")) in (0, 8):
        load_A(0, 0)
        load_A(0, 1)
        load_wd(0)
    checkpoint(6)
    dbg("aff", aff[:], [128, NCH * NE], [b_aff])
    P.emit("pool", lambda e: e.collective_compute("AllGather", ALU.bypass, replica_groups=[[0, 1, 2, 3], [4, 5, 6, 7]],
                                                 ins=[affin_d.ap().opt()], outs=[affout_d.ap().opt()]),
           [b_affin_d], [b_affout_d], extra_sem="cc_aff")
    P.barrier()
    SC.reset()
    pos = SC.take(NCH * NE, F32); b_pos = P.buf()
    L5 = SC.take(NCH * NE * 5, BF16); b_L5 = P.buf()
    colsb = [SC.take(16, F32) for _ in range(2)]; b_cols = [P.buf(), P.buf()]
    idxc = [SC.take(4, I32) for _ in range(2)]
    gatec = [SC.take(4, F32) for _ in range(2)]
    ffn_mark = SC.off
    bis = SC.take(1024, F32); b_bis = P.buf()
    bj = SC.take(1024, BF16); b_bj = P.buf()
    bs = SC.take(16, F32); b_bs = P.buf()
    for r in range(4):
        for hh in range(2):
            cidx = r * 2 + hh
            dma("sp", bis[cidx * 16:(cidx + 1) * 16, :], affout_d.ap()[r * 16:(r + 1) * 16, hh * 1024:(hh + 1) * 1024],
                [b_affout_d], [b_bis], accw=True)
    memset("dve", bs[:, 1:2], 0.5, [b_bs])
    for it in range(NBIS):
        stepn = 2.0 ** -(it + 2)
        ts("dve", bj, bis, bs[:, 1:2], None, ALU.is_ge, ALU.add, [b_bis, b_bs], [b_bj, b_bs], accum=bs[:, 0:1])
        mm(pA[:, 0:1], Gm[:], bs[:, 0:1], True, True, [b_const, b_bs], [b_pA])
        ts("dve", bs[:, 2:3], pA[:, 0:1], KTOP - 0.5, 2.0 * stepn, ALU.is_ge, ALU.mult, [b_pA], [b_bs])
        stt("dve", bs[:, 1:2], bs[:, 2:3], -stepn, bs[:, 1:2], ALU.add, ALU.add, [b_bs], [b_bs])
    ts("dve", bs[:, 3:4], bs[:, 1:2], -(2.0 ** -(NBIS + 1)), None, ALU.add, None, [b_bs], [b_bs])
    dbg("thr", bs[:, 3:4], [128, 1], [b_bs])
    thrB = SC.take(NE, F32); b_thrB = P.buf()
    thr_row = SC.take(NE, F32)
    tr(pA[0:1, 0:NE], bs[0:NE, 3:4], ident[0:NE, 0:NE], [b_bs, b_ident], [b_pA])
    cp("dve", thr_row[0:1, :], pA[0:1, 0:NE], [b_pA], [b_thrB])
    mm(pA[:, 0:NE], ones_f[0:1, :], thr_row[0:1, :], True, True, [b_thrB, b_const], [b_pA])
    cp("dve", thrB, pA[:, 0:NE], [b_pA], [b_thrB])
    mask = SC.take(NCH * NE, F32); b_mask = P.buf()
    offs = SC.take(NCH * NE, F32); b_offs = P.buf()
    mask3 = mask.rearrange("p (t e) -> p t e", e=NE)
    for c in range(NCH):
        tt("dve", mask3[:, c, :], aff3[:, c, :], thrB, ALU.is_ge, [b_aff, b_thrB], [b_mask])
    mm(pA[:, 0:256], SLT[:], mask, True, True, [b_const, b_mask], [b_pA])
    mm(pB[:, 0:256], ones_f[:], mask, True, True, [b_const, b_mask], [b_pB])
    offs3 = offs.rearrange("p (t e) -> p t e", e=NE)
    memset("dve", offs3[:, 0, :], 0.0, [b_offs])
    for c in range(1, NCH):
        tt("dve", offs3[:, c, :], offs3[:, c - 1, :], pB[:, (c - 1) * NE:c * NE], ALU.add, [b_offs, b_pB], [b_offs])
    tt("dve", pos, pA[:, 0:256], offs, ALU.add, [b_pA, b_offs], [b_pos])
    stt("dve", pos, pos, 1.0, mask, ALU.add, ALU.mult, [b_pos, b_mask], [b_pos])
    ts("dve", pos, pos, -1.0, None, ALU.add, None, [b_pos], [b_pos])
    dbg("posm", pos, [128, NCH * NE], [b_pos])
    L5v = L5.rearrange("p (t e f) -> p t e f", e=NE, f=5)
    ahi = SC.take(NCH * NE, BF16); ahif = SC.take(NCH * NE, F32); b_ah = P.buf()
    memset("dve", L5, 1.0, [b_L5])
    for c in range(NCH):
        cp("dve", L5v[:, c, :, 0], pcol[:, 0:1].broadcast_to([128, NE]), [b_const], [b_L5])
        memset("dve", L5v[:, c, :, 1], 128.0 * c, [b_L5])
    cp("dve", ahi, aff[:], [b_aff], [b_ah])
    cp("dve", ahif, ahi, [b_ah], [b_ah])
    cp("dve", L5v[:, :, :, 3], ahi.rearrange("p (t e) -> p t e", e=NE), [b_ah], [b_L5])
    tt("dve", L5v[:, :, :, 4], aff3, ahif.rearrange("p (t e) -> p t e", e=NE), ALU.subtract, [b_aff, b_ah], [b_L5])
    P.barrier()

    checkpoint(7)
    SC.off = ffn_mark
    Pe = [SC.take(CAP, BF16) for _ in range(4)]; b_Pe = [P.buf() for _ in range(4)]
    rows = SC.take(CAP, F32); b_rows = P.buf()
    xe = SC.take(3 * D, BF16); b_xe = P.buf()
    xeT = SC.take(8 * CAP, BF16); b_xeT = P.buf()
    hidT = SC.take(16 * CAP, BF16); b_hid = P.buf()
    sgl = [SC.take(CAP, F32) for _ in range(2)]; b_sgl = [P.buf(), P.buf()]
    yg = bc_t[0][:]; b_yg = b_bc[0]
    pT = pU[:].bitcast(BF16)
    b_pT = b_pU
    pG = [(pA, b_pA), (pB, b_pB)]
    pUp = [(pC, b_pC), (pD, b_pD)]
    b_pY = [P.buf(), P.buf()]
    xeT3 = xeT.rearrange("p (k s) -> p k s", k=8)
    hid3 = hidT.rearrange("p (f s) -> p f s", f=16)
    fbc = [0]

    def expert_lists(e):
        k = e % 2
        for c in range(NCH):
            q = c % 4
            ts("dve", Pe[q], iota_s[:], pos[:, c * NE + e:c * NE + e + 1], None, ALU.is_equal, None,
               [b_const, b_pos], [b_Pe[q]])
            mm(pV[0:5, 512:512 + CAP], L5v[:, c, e, :], Pe[q], c == 0, c == NCH - 1, [b_L5, b_Pe[q]], [b_pY[1]])
        cp("dve", rows[0:5, :], pV[0:5, 512:512 + CAP], [b_pY[1]], [b_rows])
        dma("sp", row_d.ap()[e], rows[0:5, :], [b_rows], [b_row_d[e]])
        c3 = colsb[k][:, 0:15].rearrange("p (f j) -> p f j", j=3)
        dma("sp", c3, row_d.ap()[e].rearrange("f (p j) -> p f j", j=3), [b_row_d[e]], [b_cols[k]])
        tt("dve", gatec[k][:, 0:3], c3[:, 3, :], c3[:, 4, :], ALU.add, [b_cols[k]], [b_cols[k]])
        tt("dve", c3[:, 0, :], c3[:, 0, :], c3[:, 1, :], ALU.add, [b_cols[k]], [b_cols[k]])
        ts("dve", c3[:, 2, :], c3[:, 2, :], -1.0, -1.0, ALU.add, ALU.mult, [b_cols[k]], [b_cols[k]])
        tt("dve", c3[:, 2, :], c3[:, 2, :], basecol[:], ALU.mult, [b_cols[k], b_const], [b_cols[k]])
        tt("dve", c3[:, 0, :], c3[:, 0, :], c3[:, 2, :], ALU.add, [b_cols[k]], [b_cols[k]])
        cp("dve", idxc[k][:, 0:3], c3[:, 0, :], [b_cols[k]], [b_cols[k]])
        if debug and e == 0:
            dbg("idx0", c3[:, 0, :], [128, 3], [b_cols[k]])
            dbg("gate0", gatec[k][:, 0:3], [128, 3], [b_cols[k]])
        for j in range(3):
            P.emit("pool", (lambda k_, j_: (lambda e_: e_.indirect_dma_start(
                out=xe[:, j_ * D:(j_ + 1) * D], out_offset=None, in_=h2_d.ap(),
                in_offset=bass.IndirectOffsetOnAxis(ap=idxc[k_][:, j_:j_ + 1], axis=0))))(k, j),
                [b_cols[k], b_h2_d], [b_xe], dma=True, accw=(j > 0))

    def tr_b(out, in_, rd, wr):
        P.emit("pe", lambda e_: e_.transpose(out=out, in_=in_, identity=ident_b[:]), rd, wr)

    expert_lists(0)
    for e in range(NE):
        k = e % 2
        for j in range(3):
            for kc in range(8):
                tr_b(pT[:, kc * 128:(kc + 1) * 128], xe[:, j * D + kc * 128:j * D + (kc + 1) * 128], [b_xe, b_ident], [b_pT])
            act(xeT3[:, :, j * 128:(j + 1) * 128], pT[:, 0:1024].rearrange("p (k s) -> p k s", k=8), AF.Copy, [b_pT], [b_xeT])
        for fb in range(4):
            s_ = fb % 2
            for fc in range(4):
                kk = fc % 2
                pg, b_pg = pG[kk]
                pu, b_pu = pUp[kk]
                for kc in range(8):
                    mm(pg[:, 0:CAP], gu[s_][0][:, kc, fc * 128:(fc + 1) * 128], xeT3[:, kc, :], kc == 0, kc == 7,
                       [b_gu[s_][0], b_xeT], [b_pg])
                for kc in range(8):
                    mm(pu[:, 0:CAP], gu[s_][1][:, kc, fc * 128:(fc + 1) * 128], xeT3[:, kc, :], kc == 0, kc == 7,
                       [b_gu[s_][1], b_xeT], [b_pu])
                act(sgl[kk], pg[:, 0:CAP], AF.Silu, [b_pg], [b_sgl[kk]])
                tt("dve", hid3[:, fb * 4 + fc, :], sgl[kk], pu[:, 0:CAP], ALU.mult, [b_sgl[kk], b_pu], [b_hid])
            if fb + 2 < 4:
                load_A(e, fb + 2)
            if fb == 1 and e + 1 < NE:
                expert_lists(e + 1)
        if e + 1 < NE:
            load_A(e + 1, 0)
            load_A(e + 1, 1)
        for j in range(3):
            for half in range(2):
                for f in range(16):
                    mm(pV[:, half * 512:(half + 1) * 512], hid3[:, f, j * 128:(j + 1) * 128],
                       wd_sb[:, f, half * 512:(half + 1) * 512], f == 0, f == 15, [b_hid, b_wd], [b_pY[half]])
            act(yg[:, 0:512], pV[:, 0:512], AF.Copy, [b_pY[0], b_cols[k]], [b_yg], scale=gatec[k][:, j:j + 1])
            ts("dve", yg[:, 512:1024], pV[:, 512:1024], gatec[k][:, j:j + 1], None, ALU.mult, None,
               [b_pY[1], b_cols[k], b_yg], [b_yg])
            P.emit("pool", (lambda k_, j_: (lambda e_: e_.indirect_dma_start(
                out=acc_d.ap(), out_offset=bass.IndirectOffsetOnAxis(ap=idxc[k_][:, j_:j_ + 1], axis=0),
                in_=yg, in_offset=None, compute_op=ALU.add)))(k, j),
                [b_yg, b_cols[k], b_acc_d], [b_acc_d], dma=True)
        if e + 1 < NE:
            load_wd(e + 1)
    P.barrier()

    checkpoint(8)
    SC.reset()
    bc_load(2, mod_d.ap()[0:1, 5 * D:6 * D])
    bc_load(0, fg_d.ap()[0:1, :])
    ac = [SC.take(D, F32) for _ in range(4)]; b_ac = [P.buf() for _ in range(4)]
    ot = [SC.take(D, F32) for _ in range(2)]; b_ot = [P.buf(), P.buf()]
    junk3 = [SC.take(D, BF16) for _ in range(2)]; b_junk3 = [P.buf(), P.buf()]
    st3 = [SC.take(8, F32) for _ in range(2)]; b_st3 = [P.buf(), P.buf()]
    def fin_load(c_):
        dma("poolq", ac[c_ % 4], acc_d.ap()[c_ * 128:(c_ + 1) * 128, :], [b_acc_d], [b_ac[c_ % 4]])

    for c_ in range(3):
        fin_load(c_)
    for c in range(NCH):
        k = c % 4
        k2 = c % 2
        if c + 3 < NCH:
            fin_load(c + 3)
        tt("dve", ac[k], ac[k], bc_t[2][:], ALU.mult, [b_ac[k], b_bc[2]], [b_ac[k]])
        tt("dve", ac[k], ac[k], x1_tile(c), ALU.add, [b_ac[k], b_x1[c]], [b_ac[k]])
        act(junk3[k2], ac[k], AF.Square, [b_ac[k]], [b_junk3[k2], b_st3[k2]], accum_out=st3[k2][:, 0:1])
        act(st3[k2][:, 1:2], st3[k2][:, 0:1], AF.Ln, [b_st3[k2], b_const], [b_st3[k2]], scale=1.0 / D, bias=cst[:, 1:2])
        act(st3[k2][:, 2:3], st3[k2][:, 1:2], AF.Exp, [b_st3[k2]], [b_st3[k2]], scale=-0.5)
        stt("dve", ot[k2], ac[k], st3[k2][:, 2:3], bc_t[0][:], ALU.mult, ALU.mult, [b_ac[k], b_st3[k2], b_bc[0]], [b_ot[k2]])
        dma("sp", out_d.ap()[c * 128:(c + 1) * 128, :], ot[k2], [b_ot[k2]], [b_out_d], accw=True)


_CACHE = {}


def _NWE():
    return 1 if int(os.environ.get("MK_STOP", "0")) not in (0, 7, 8) else NE


def make_maps(inp):
    f32 = lambda a: np.ascontiguousarray(np.asarray(a, dtype=np.float32))
    x = f32(inp["x"]); c = f32(inp["c"]); ctx = f32(inp["ctx"]); c_ctx = f32(inp["c_ctx"])
    shared = {
        "w_ada": f32(inp["w_ada"][0]),
        "b_ada": f32(inp["b_ada"][0]).reshape(1, 6 * D),
        "norm1_g": f32(inp["norm1_g"][0]).reshape(1, D),
        "norm2_g": f32(inp["norm2_g"][0]).reshape(1, D),
        "w_in": f32(inp["w_in"][0]),
        "w_a_up": f32(inp["gla_w_a_up"][0]).reshape(32, 512),
        "b_a": f32(inp["gla_b_a"][0]).reshape(2, 512),
        "gn_col": f32(f32(inp["gla_norm_g"][0]).reshape(2, 128).T),
        "gla_w_o": f32(inp["gla_w_o"][0]),
        "cw_col": f32(f32(inp["conv_w"][0]).reshape(3, 8, 128).transpose(2, 0, 1).reshape(128, 24)),
        "conv_w_out": f32(inp["conv_w_out"][0]),
        "merge_w_out": f32(inp["merge_w_out"][0]),
        "router_w": f32(inp["router_w"][0]),
        "exp_w_gate": f32(inp["exp_w_gate"][0][:_NWE()]),
        "exp_w_up": f32(inp["exp_w_up"][0][:_NWE()]),
        "exp_w_down": f32(inp["exp_w_down"][0][:_NWE()]),
        "final_g": f32(inp["final_g"]).reshape(1, D),
    }
    maps = []
    for core in range(8):
        b, r = divmod(core, 4)
        cc = np.concatenate([c[b].reshape(8, 128).T, c_ctx.reshape(8, 128).T], axis=1)
        cm = np.zeros((128, 8), np.float32)
        for q in range(4):
            cm[:, q] = 1.0 if q < r else 0.0
            cm[:, 4 + q] = 1.0 if q > r else 0.0
        m = dict(shared)
        m["x"] = f32(x[b, r * NT:(r + 1) * NT])
        m["ctx"] = f32(ctx[b])
        m["cc"] = f32(cc)
        m["cmask"] = cm
        maps.append(m)
    return maps


def kernel(**inputs):
    nc = build_program(debug=False)[0]
    maps = make_maps(inputs)
    res = run_bass_kernel_spmd(nc, maps, core_ids=list(range(8)))
    out = np.empty((2, 4 * NT, D), np.float32)
    for core in range(8):
        b, r = divmod(core, 4)
        out[b, r * NT:(r + 1) * NT] = np.asarray(res.results[core]["out"], dtype=np.float32)
    return out
```

```python
import contextlib
import os
import numpy as np
import concourse.bass as bass
import concourse.mybir as mybir
from concourse.bass_utils import run_bass_kernel_spmd

F32 = mybir.dt.float32
BF16 = mybir.dt.bfloat16
I32 = mybir.dt.int32
ALU = mybir.AluOpType
AF = mybir.ActivationFunctionType
AX = mybir.AxisListType

NT = 2048
NCH = 16
D = 1024
CAP = 384
NROW = NT + CAP
NE = 16
KTOP = 1024
NBIS = 25
EPS = 1e-6
O_Q, O_K, O_V, O_G, O_LRF, O_LRB, O_U, O_BG, O_CG, O_RG, O_RC = (
    0, 512, 1024, 2048, 3072, 3088, 3104, 4128, 5152, 6176, 7200)
DEBUG = bool(int(os.environ.get("MK_DEBUG", "0")))


class Buf:
    def __init__(self, name):
        self.name = name
        self.w = []
        self.r = []


class Prog:
    ENGS = ("pe", "act", "dve", "pool", "sp")
    NDMA = {"sp": 14, "act": 8, "pool": 14}

    def __init__(self, nc):
        self.nc = nc
        self.stack = contextlib.ExitStack()
        self.q = {e: [] for e in self.ENGS}
        self.sems = {}
        self.cnt = {}
        self.known = {e: {} for e in self.ENGS}
        for e in self.ENGS:
            self._sem("c_" + e)
        self.dma_rr = {e: 0 for e in self.NDMA}
        for e, n in self.NDMA.items():
            for i in range(n):
                self._sem("d_%s%d" % (e, i))
        self.nbuf = 0

    def _sem(self, key):
        self.sems[key] = self.stack.enter_context(self.nc.semaphore(key))
        self.cnt[key] = 0

    def sb(self, name, shape, dtype=F32):
        return self.stack.enter_context(self.nc.sbuf_tensor("s_" + name, list(shape), dtype))

    def ps(self, name, shape, dtype=F32):
        return self.stack.enter_context(self.nc.psum_tensor("p_" + name, list(shape), dtype))

    def buf(self, name=None):
        self.nbuf += 1
        return Buf(name or "b%d" % self.nbuf)

    def emit(self, eng, fn, reads=(), writes=(), dma=False, extra_sem=None, accw=False):
        deps = {}

        def add(tok):
            if tok is None:
                return
            k, v = tok
            if deps.get(k, 0) < v:
                deps[k] = v

        for b in reads:
            for t in b.w:
                add(t)
        for b in writes:
            if not accw:
                for t in b.w:
                    add(t)
            for t in b.r:
                add(t)
        if dma:
            i = self.dma_rr[eng]
            self.dma_rr[eng] = (i + 1) % self.NDMA[eng]
            key = "d_%s%d" % (eng, i)
            add((key, self.cnt[key]))
            self.cnt[key] += 16
            tok = (key, self.cnt[key])
            inc = 16
        elif extra_sem is not None:
            key = extra_sem
            self._sem(key)
            self.cnt[key] = 1
            tok = (key, 1)
            inc = None
        else:
            key = "c_" + eng
            self.cnt[key] += 1
            tok = (key, self.cnt[key])
            inc = 1
        waits = []
        kn = self.known[eng]
        own = "c_" + eng
        for k, v in deps.items():
            if v <= 0:
                continue
            if k == own and eng == "pe":
                continue
            if kn.get(k, 0) >= v:
                continue
            kn[k] = v
            waits.append((k, v))
        self.q[eng].append((waits, fn, key, inc))
        for b in writes:
            if accw:
                b.w.append(tok)
            else:
                b.w = [tok]
            b.r = []
        for b in reads:
            b.r.append(tok)
        return tok

    def emit_wait(self, eng, toks):
        waits = []
        kn = self.known[eng]
        for k, v in toks:
            if v <= 0 or kn.get(k, 0) >= v:
                continue
            kn[k] = v
            waits.append((k, v))
        if waits:
            self.q[eng].append((waits, None, None, None))

    def barrier(self):
        toks = [(k, v) for k, v in self.cnt.items() if v > 0]
        for e in self.ENGS:
            self.emit_wait(e, toks)

    def build(self):
        nc = self.nc
        with nc.Block() as block:

            def run(name):
                def f(e):
                    for waits, fn, key, inc in self.q[name]:
                        for k, v in waits:
                            e.wait_ge(self.sems[k], v)
                        if fn is None:
                            continue
                        ins = fn(e)
                        if inc is None:
                            ins.then_inc(self.sems[key])
                        else:
                            ins.then_inc(self.sems[key], inc)
                return f

            block.tensor(run("pe"))
            block.scalar(run("act"))
            block.vector(run("dve"))
            block.gpsimd(run("pool"))
            block.sync(run("sp"))
        self.stack.close()


class Arena:
    def __init__(self, t, nwords):
        self.t = t
        self.n = nwords
        self.off = 0

    def reset(self):
        self.off = 0

    def take(self, nelem, dtype=F32):
        words = nelem if dtype in (F32, I32) else (nelem + 1) // 2
        a = self.off
        self.off += words
        assert self.off <= self.n, ("arena overflow", self.off, self.n)
        v = self.t[:, a:a + words]
        if dtype != F32:
            v = v.bitcast(dtype)
        return v


class _Stop(Exception):
    pass


def build_program(debug=False):
    nc = bass.Bass("TRN2", target_bir_lowering=False)
    P = Prog(nc)
    dbg_out = {}
    stop_at = int(os.environ.get("MK_STOP", "0"))

    def checkpoint(n):
        if stop_at == n:
            raise _Stop()

    try:
        _emit_all(nc, P, dbg_out, debug, checkpoint)
    except _Stop:
        pass
    P.emit_wait("sp", [(k_, v_) for k_, v_ in P.cnt.items() if v_ > 0])
    P.build()
    return nc, dbg_out


def _emit_all(nc, P, dbg_out, debug, checkpoint):

    def din(name, shape, dt=F32):
        return nc.dram_tensor(name, list(shape), dt, kind="ExternalInput")

    x_d = din("x", [NT, D])
    ctx_d = din("ctx", [256, D])
    cc_d = din("cc", [128, 16])
    cm_d = din("cmask", [128, 8])
    wada_d = din("w_ada", [D, 6 * D])
    bada_d = din("b_ada", [1, 6 * D])
    n1_d = din("norm1_g", [1, D])
    n2_d = din("norm2_g", [1, D])
    win_d = din("w_in", [D, 8224])
    wup_d = din("w_a_up", [32, 512])
    ba_d = din("b_a", [2, 512])
    gn_d = din("gn_col", [128, 2])
    wo_d = din("gla_w_o", [D, D])
    cw_d = din("cw_col", [128, 24])
    cwo_d = din("conv_w_out", [D, D])
    mw_d = din("merge_w_out", [D, D])
    rw_d = din("router_w", [D, NE])
    n_we = 1 if int(os.environ.get("MK_STOP", "0")) not in (0, 7, 8) else NE
    wg_d = din("exp_w_gate", [n_we, D, 2 * D])
    wu_d = din("exp_w_up", [n_we, D, 2 * D])
    wd_d = din("exp_w_down", [n_we, 2 * D, D])
    fg_d = din("final_g", [1, D])
    out_d = nc.dram_tensor("out", [NT, D], F32, kind="ExternalOutput")
    mod_d = nc.dram_tensor("mod_s", [2, 6 * D], F32)
    agin_p = [nc.dram_tensor("ag_in%d" % i, [128, 1028], F32) for i in range(2)]
    agout_p = [nc.dram_tensor("ag_out%d" % i, [512, 1028], F32) for i in range(2)]
    b_agin_p = [P.buf() for _ in range(2)]
    b_agout_p = [P.buf() for _ in range(2)]
    h2_d = nc.dram_tensor("h2_s", [NROW, D], BF16)
    acc_d = nc.dram_tensor("acc_s", [NROW, D], F32)
    affin_d = nc.dram_tensor("aff_in", [NE, NT], F32)
    affout_d = nc.dram_tensor("aff_out", [4 * NE, NT], F32)
    row_d = nc.dram_tensor("row_s", [NE, 5, CAP], F32)
    b_mod_d, b_agin_d, b_agout_d, b_h2_d, b_acc_d, b_affin_d, b_affout_d, b_out_d = [P.buf() for _ in range(8)]
    b_row_d = [P.buf() for _ in range(NE)]

    b_dbg = P.buf()

    def dbg(name, ap, shape, rd, dt=F32):
        if not debug:
            return
        t = nc.dram_tensor("dbg_" + name, list(shape), dt, kind="ExternalOutput")
        dbg_out[name] = t
        P.emit("sp", lambda e: e.dma_start(out=t.ap(), in_=ap), rd, [b_dbg], dma=True, accw=True)

    def mm(out, lhsT, rhs, st, sp_, rd, wr):
        P.emit("pe", lambda e: e.matmul(out, lhsT=lhsT, rhs=rhs, start=st, stop=sp_), rd, wr)

    def tr(out, in_, ident, rd, wr):
        P.emit("pe", lambda e: e.transpose(out=out, in_=in_, identity=ident), rd, wr)

    def act(out, in_, func, rd, wr, **kw):
        P.emit("act", lambda e: e.activation(out=out, in_=in_, func=func, **kw), rd, wr)

    def tt(eng, out, in0, in1, op, rd, wr):
        P.emit(eng, lambda e: e.tensor_tensor(out=out, in0=in0, in1=in1, op=op), rd, wr)

    def ts(eng, out, in0, s1, s2, op0, op1, rd, wr, accum=None):
        if accum is None:
            if s2 is None:
                P.emit(eng, lambda e: e.tensor_scalar(out=out, in0=in0, scalar1=s1, scalar2=None, op0=op0), rd, wr)
            else:
                P.emit(eng, lambda e: e.tensor_scalar(out=out, in0=in0, scalar1=s1, scalar2=s2, op0=op0, op1=op1), rd, wr)
        else:
            P.emit(eng, lambda e: e.tensor_scalar(out=out, in0=in0, scalar1=s1, scalar2=s2, op0=op0, op1=op1,
                                                 accum_out=accum), rd, wr)

    def stt(eng, out, in0, scalar, in1, op0, op1, rd, wr):
        eng = "dve"
        P.emit(eng, lambda e: e.scalar_tensor_tensor(out=out, in0=in0, scalar=scalar, in1=in1, op0=op0, op1=op1), rd, wr)

    def cp(eng, out, in_, rd, wr):
        P.emit(eng, lambda e: e.tensor_copy(out=out, in_=in_), rd, wr)

    def dma(eng, out, in_, rd, wr, accw=False, slow=False):
        q_ = "pool" if eng == "poolq" else "sp"
        if slow:
            P.emit(q_, lambda e: e.dma_start(out=out, in_=in_, allow_slow_non_contiguous=True), rd, wr, dma=True, accw=accw)
        else:
            P.emit(q_, lambda e: e.dma_start(out=out, in_=in_), rd, wr, dma=True, accw=accw)

    def memset(eng, ap, val, wr):
        P.emit(eng, lambda e: e.memset(ap, val), (), wr)

    def asel(out, in_, pattern, op, fill, base, cm, rd, wr):
        P.emit("pool", lambda e: e.affine_select(out=out, in_=in_, pattern=pattern, compare_op=op, fill=fill,
                                                 base=base, channel_multiplier=cm), rd, wr)

    def load_w(dst, src2d, wr, rd=()):
        P.emit("pool", lambda e: e.dma_start(out=dst, in_=src2d.rearrange("(k p) n -> p k n", p=128)), rd, wr, dma=True)

    R0f = P.sb("R0", [128, 8192], F32)
    R1 = P.sb("R1", [128, 16384], BF16)
    R2 = P.sb("R2", [128, 16384], BF16)
    R3f = P.sb("R3", [128, 8192], F32)
    SCt = P.sb("SC", [128, 9216], F32)
    SC = Arena(SCt, 9216)
    R0b = R0f[:].bitcast(BF16)
    R3b = R3f[:].bitcast(BF16)
    hT = R0b.rearrange("p (k t) -> p k t", k=8)
    b_hT = [P.buf("hT%d" % i) for i in range(NCH)]
    w4 = lambda R, i: R[:, i * 4096:(i + 1) * 4096].rearrange("p (k n) -> p k n", k=8)

    ident = P.sb("ident", [128, 128], F32); b_ident = P.buf()
    ident_b = P.sb("ident_b", [128, 128], BF16)
    TIf = P.sb("TIf", [128, 128], F32); TIb = P.sb("TIb", [128, 128], F32)
    TCf = P.sb("TCf", [128, 128], F32); TCb = P.sb("TCb", [128, 128], F32)
    SLT = P.sb("SLT", [128, 128], F32)
    Mf4 = P.sb("Mf4", [128, 512], F32); Mb4 = P.sb("Mb4", [128, 512], F32)
    ones_f = P.sb("ones_f", [128, 128], F32)
    Gm = P.sb("Gm", [128, 128], F32)
    b_const = P.buf("const")
    cst = P.sb("cst", [128, 4], F32)
    iota_s = P.sb("iota_s", [128, CAP], F32)
    basecol = P.sb("basecol", [128, 3], F32)
    pcol = P.sb("pcol", [128, 1], F32)
    wlr = P.sb("wlr", [128, 8, 32], BF16); b_wlr = P.buf()
    wupa = [P.sb("wupa%d" % i, [17, 512], F32) for i in range(2)]; b_wupa = P.buf()
    lrTa = [P.sb("lrTa%d" % i, [17, 128], F32) for i in range(2)]; b_lrTa = [P.buf(), P.buf()]
    gn = P.sb("gn", [128, 2], F32)
    cwc = P.sb("cwc", [128, 24], F32)
    cmask = P.sb("cmask", [128, 8], F32)
    bc_t = [P.sb("bc%d" % i, [128, D], F32) for i in range(3)]; b_bc = [P.buf() for _ in range(3)]
    aff = P.sb("aff", [128, NCH * NE], F32); b_aff = P.buf()
    wa2 = P.sb("wa2", [128, 8, 512], BF16); b_wa2 = P.buf()
    mrow2 = P.sb("mrow2", [2, 512], F32); brow2 = P.sb("brow2", [2, 512], F32); b_mrow2 = P.buf(); b_brow2 = P.buf()
    scc_p = P.sb("scc_p", [128, 16], BF16)
    Sinb_bf = P.sb("Sinb_bf", [128, 1024], BF16); b_Sinb_bf = P.buf()
    dB_all = P.sb("dB_all", [128, NCH * 4], F32); b_dB = P.buf()
    DcB = P.sb("DcB", [128, NCH * 4], F32); b_DcB = P.buf()

    pA = P.ps("pA", [128, 512]); pB = P.ps("pB", [128, 512]); pC = P.ps("pC", [128, 512]); pD = P.ps("pD", [128, 512])
    pV = P.ps("pV", [128, 1024]); pU = P.ps("pU", [128, 1024])
    b_pA, b_pB, b_pC, b_pD, b_pV, b_pU = [P.buf(n) for n in ("pA", "pB", "pC", "pD", "pV", "pU")]

    b_wa = [P.buf() for _ in range(4)]
    for cb_ in range(4):
        load_w(w4(R1, cb_), wada_d.ap()[:, cb_ * 512:(cb_ + 1) * 512], [b_wa[cb_]])
    cc_sb = SC.take(16, F32); scc = scc_p[:]; b_cc = P.buf()
    dma("sp", cc_sb, cc_d.ap(), (), [b_cc])
    iot_i = SC.take(128 * 0 + CAP, I32)
    tmp_i = SC.take(128, I32)
    b_tmp = P.buf()
    P.emit("pool", lambda e: e.memset(ident[:], 1.0), (), [b_ident])
    asel(ident[:], ident[:], [[-1, 128]], ALU.is_equal, 0.0, 0, 1, [b_ident], [b_ident])
    cp("dve", ident_b[:], ident[:], [b_ident], [b_ident])
    CND = {"ge": (-1, 1, ALU.is_ge), "gt": (-1, 1, ALU.is_gt), "le": (1, -1, ALU.is_ge), "lt": (1, -1, ALU.is_gt)}
    for t_, cnd, val in ((TIf, "le", -1.0 / 16), (TIb, "ge", -1.0 / 16), (TCf, "gt", -1.0 / 16),
                         (TCb, "lt", -1.0 / 16), (SLT, "lt", 1.0)):
        st_, cm_, op_ = CND[cnd]
        memset("pool", t_[:], val, [b_const])
        asel(t_[:], t_[:], [[st_, 128]], op_, 0.0, 0, cm_, [b_const], [b_const])
    for t_, cnd in ((Mf4, "le"), (Mb4, "ge")):
        st_, cm_, op_ = CND[cnd]
        memset("pool", t_[:], 1.0, [b_const])
        v = t_[:].rearrange("p (h j) -> p h j", h=4)
        asel(v, v, [[0, 4], [st_, 128]], op_, 0.0, 0, cm_, [b_const], [b_const])
    memset("pool", ones_f[:], 1.0, [b_const])
    memset("pool", cst[:, 0:1], 1.0, [b_const])
    memset("pool", cst[:, 1:2], EPS, [b_const])
    memset("pool", cst[:, 2:3], -1.0 / 16, [b_const])
    memset("pool", cst[:, 3:4], 0.0, [b_const])
    for i in range(2):
        memset("pool", lrTa[i][:], 1.0, [b_lrTa[i]])
    P.emit("pool", lambda e: e.iota(iot_i, pattern=[[1, CAP]], base=0, channel_multiplier=0), (), [b_tmp])
    cp("dve", iota_s[:], iot_i, [b_tmp], [b_const])
    P.emit("pool", lambda e: e.iota(iot_i[:, 0:3], pattern=[[1, 3]], base=NT, channel_multiplier=3), (), [b_tmp])
    cp("dve", basecol[:], iot_i[:, 0:3], [b_tmp], [b_const])
    P.emit("pool", lambda e: e.iota(iot_i[:, 0:1], pattern=[[0, 1]], base=0, channel_multiplier=1), (), [b_tmp])
    cp("dve", pcol[:], iot_i[:, 0:1], [b_tmp], [b_const])
    P.emit("pool", lambda e: e.iota(tmp_i, pattern=[[-1, 128]], base=128, channel_multiplier=1), (), [b_tmp])
    P.emit("dve", lambda e: e.tensor_single_scalar(out=tmp_i, in_=tmp_i, scalar=15, op=ALU.bitwise_and), [b_tmp], [b_tmp])
    P.emit("dve", lambda e: e.tensor_single_scalar(out=Gm[:], in_=tmp_i, scalar=0, op=ALU.is_equal), [b_tmp], [b_const])
    dma("sp", gn[:], gn_d.ap(), (), [b_const])
    dma("sp", cwc[:], cw_d.ap(), (), [b_const])
    dma("sp", cmask[:], cm_d.ap(), (), [b_const])
    for i in range(2):
        dma("sp", wupa[i][0:16, :], wup_d.ap()[i * 16:(i + 1) * 16, :], (), [b_wupa])
        dma("sp", wupa[i][16:17, :], ba_d.ap()[i:i + 1, :], (), [b_wupa])
    P.emit("pool", lambda e: e.dma_start(out=wlr[:], in_=win_d.ap()[:, O_LRF:O_LRF + 32].rearrange("(k p) n -> p k n", p=128)),
           (), [b_wlr], dma=True)
    mrow = SC.take(512, F32); brow = SC.take(512, F32); b_mrow = P.buf(); b_brow = P.buf()
    act(scc, cc_sb, AF.Silu, [b_cc], [b_cc])

    def adaln_block(cb):
        slot = w4(R1, cb % 4)
        if cb >= 4:
            load_w(slot, wada_d.ap()[:, cb * 512:(cb + 1) * 512], [b_wa[cb % 4]])
        for i in range(2):
            dma("sp", brow[i:i + 1, :], bada_d.ap()[0:1, cb * 512:(cb + 1) * 512], (), [b_brow])
        for kc in range(8):
            mm(pA[0:2, :], scc[:, kc:16:8], slot[:, kc, :], kc == 0, kc == 7, [b_cc, b_wa[cb % 4]], [b_pA])
        tt("dve", mrow[0:2, :], pA[0:2, :], brow[0:2, :], ALU.add, [b_pA, b_brow], [b_mrow])
        dma("sp", mod_d.ap()[:, cb * 512:(cb + 1) * 512], mrow[0:2, :], [b_mrow], [b_mod_d])

    for cb in range(4):
        adaln_block(cb)

    def adaln_block_late(cb):
        load_w(wa2[:], wada_d.ap()[:, cb * 512:(cb + 1) * 512], [b_wa2])
        for i in range(2):
            dma("sp", brow2[i:i + 1, :], bada_d.ap()[0:1, cb * 512:(cb + 1) * 512], (), [b_brow2])
        for kc in range(8):
            mm(pA[0:2, :], scc[:, kc:16:8], wa2[:, kc, :], kc == 0, kc == 7, [b_cc, b_wa2], [b_pA])
        tt("dve", mrow2[0:2, :], pA[0:2, :], brow2[0:2, :], ALU.add, [b_pA, b_brow2], [b_mrow2])
        dma("sp", mod_d.ap()[:, cb * 512:(cb + 1) * 512], mrow2[0:2, :], [b_mrow2], [b_mod_d])

    checkpoint(1)
    def bc_load(i, src_row):
        dma("sp", bc_t[i][:], src_row.broadcast_to([128, D]), [b_mod_d], [b_bc[i]])

    def make_AB(row, sc_off, sh_off, ng_d):
        bc_load(0, mod_d.ap()[row:row + 1, sc_off:sc_off + D])
        bc_load(2, ng_d.ap()[0:1, :])
        stt("dve", bc_t[0][:], bc_t[0][:], 1.0, bc_t[2][:], ALU.add, ALU.mult, [b_bc[0], b_bc[2]], [b_bc[0]])
        bc_load(1, mod_d.ap()[row:row + 1, sh_off:sh_off + D])

    ND1 = 3
    xt = [SC.take(D, F32) for _ in range(ND1)]; b_xt = [P.buf() for _ in range(ND1)]
    hf = [SC.take(D, F32) for _ in range(ND1)]; b_hf = [P.buf() for _ in range(ND1)]
    junk_ = [SC.take(D, BF16) for _ in range(2)]; b_junk_ = [P.buf(), P.buf()]
    stat_ = [SC.take(8, F32) for _ in range(ND1)]; b_stat_ = [P.buf() for _ in range(ND1)]

    def norm_tile(src, b_src, i, A, b_A, B, b_B):
        junk, b_junk, stat, b_stat = junk_[i % 2], b_junk_[i % 2], stat_[i], b_stat_[i]
        act(junk, src, AF.Square, [b_src], [b_junk, b_stat], accum_out=stat[:, 0:1])
        act(stat[:, 1:2], stat[:, 0:1], AF.Ln, [b_stat, b_const], [b_stat], scale=1.0 / D, bias=cst[:, 1:2])
        act(stat[:, 2:3], stat[:, 1:2], AF.Exp, [b_stat], [b_stat], scale=-0.5)
        stt("dve", hf[i], src, stat[:, 2:3], A, ALU.mult, ALU.mult, [b_src, b_stat, b_A], [b_hf[i]])
        if B is not None:
            tt("dve", hf[i], hf[i], B, ALU.add, [b_hf[i], b_B], [b_hf[i]])

    tcount = [0]

    def transpose_to(i, dst3, b_dst, t0):
        pX, b_pX = (pV, b_pV) if tcount[0] % 2 == 0 else (pU, b_pU)
        tcount[0] += 1
        for kc in range(8):
            tr(pX[:, kc * 128:(kc + 1) * 128], hf[i][:, kc * 128:(kc + 1) * 128], ident[:], [b_hf[i], b_ident], [b_pX])
        act(dst3[:, :, t0:t0 + 128], pX[:].rearrange("p (k t) -> p k t", k=8), AF.Copy, [b_pX], [b_dst])

    wq, wk, wv0, wv1 = [w4(R2, i) for i in range(4)]
    b_wq, b_wk, b_wv = P.buf(), P.buf(), P.buf()
    load_w(wk, win_d.ap()[:, O_K:O_K + 512], [b_wk])
    load_w(wv0, win_d.ap()[:, O_V:O_V + 512], [b_wv])
    load_w(wv1, win_d.ap()[:, O_V + 512:O_V + 1024], [b_wv])
    load_w(wq, win_d.ap()[:, O_Q:O_Q + 512], [b_wq])

    make_AB(1, 1 * D, 0 * D, n1_d)
    cT = R3b[:, 8192:8192 + 2048].rearrange("p (k t) -> p k t", k=8)
    b_cT = P.buf()
    for i in range(2):
        dma("poolq", xt[i], ctx_d.ap()[i * 128:(i + 1) * 128, :], (), [b_xt[i]])
        norm_tile(xt[i], b_xt[i], i, bc_t[0][:], b_bc[0], bc_t[1][:], b_bc[1])
        transpose_to(i, cT, b_cT, i * 128)
    make_AB(0, 1 * D, 0 * D, n1_d)

    def n1_stage1(c_):
        i_ = c_ % ND1
        dma("poolq", xt[i_], x_d.ap()[c_ * 128:(c_ + 1) * 128, :], (), [b_xt[i_]])
        norm_tile(xt[i_], b_xt[i_], i_, bc_t[0][:], b_bc[0], bc_t[1][:], b_bc[1])

    n1_stage1(0)
    for c in range(NCH):
        if c + 1 < NCH:
            n1_stage1(c + 1)
        transpose_to(c % ND1, hT, b_hT[c], c * 128)
    if debug:
        hdb = hf[1]; b_hdb = b_hf[1]
        cp("dve", hdb, hT[:, :, 0:128], [b_hT[0]], [b_hdb])
        dbg("hT0", hdb, [128, D], [b_hdb])
    checkpoint(2)
    P.barrier()
    zt = bc_t[1][:]
    b_zt = b_bc[1]
    memset("dve", zt, 0.0, [b_zt])
    for i in range(NROW // 128):
        dma("sp", acc_d.ap()[i * 128:(i + 1) * 128, :], zt, [b_zt], [b_acc_d], accw=True)
    ztb = zt.bitcast(BF16)[:, 0:D]
    for i in range(CAP // 128):
        dma("sp", h2_d.ap()[NT + i * 128:NT + (i + 1) * 128, :], ztb, [b_zt], [b_h2_d], accw=True)

    SC.reset()
    l_sb = [SC.take(512, F32) for _ in range(2)]; b_l = [P.buf(), P.buf()]
    e_tmp = SC.take(512, F32); b_etmp = P.buf()
    v_bf = SC.take(1024, BF16); b_v = P.buf()
    edec = SC.take(512, F32); b_edec = P.buf()
    kdec = [SC.take(512, BF16) for _ in range(2)]; b_kdec = [P.buf(), P.buf()]
    sm = SC.take(32, F32); b_sm = P.buf()
    S_f = SC.take(1024, F32); b_Sf = P.buf()
    sc_mark = SC.off
    S_b = [SC.take(1024, F32) for _ in range(3)]; b_Sb = [P.buf() for _ in range(3)]
    SC.off = sc_mark
    eb = SC.take(512, F32); enb = SC.take(512, F32); b_eb = P.buf(); b_enb = P.buf()
    qin = [SC.take(512, BF16) for _ in range(2)]; kin = [SC.take(512, BF16) for _ in range(2)]
    b_qin = [P.buf(), P.buf()]; b_kin = [P.buf(), P.buf()]
    scm = [SC.take(512, BF16) for _ in range(2)]; b_scm = [P.buf(), P.buf()]
    Sf_bf = SC.take(1024, BF16); b_Sfbf = P.buf()
    on = SC.take(1024, F32); b_on = P.buf()
    qinD = SC.take(512, BF16); b_qinD = P.buf()
    Gst = R3f[:, 0:1032]; b_G = P.buf()
    Gst2 = [R3f[:, 0:1028], R3f[:, 1032:2060]]; b_G2 = [b_G, P.buf()]
    tmpS = R3f[:, 2064:3088]; b_tmpS = P.buf()
    sctx = [R3f[:, 6144:7168], R3f[:, 7168:8192]]; b_sctx = [P.buf(), P.buf()]
    R1v = R1[:].rearrange("p (c n) -> p c n", c=NCH)
    b_R1 = [P.buf("R1_%d" % c) for c in range(NCH)]
    d_f = sm[:, 0:4]; d_b = sm[:, 4:8]

    def chunk_kvl(src3, b_src, t0, need_b):
        for kc in range(8):
            mm(pC[:], src3[:, kc, t0:t0 + 128], wk[:, kc, :], kc == 0, kc == 7, [b_src, b_wk], [b_pC])
        for hv, wv in enumerate((wv0, wv1)):
            for kc in range(8):
                mm(pV[:, hv * 512:(hv + 1) * 512], src3[:, kc, t0:t0 + 128], wv[:, kc, :], kc == 0, kc == 7,
                   [b_src, b_wv], [b_pV])
        for i in range(2):
            for kc in range(8):
                mm(pD[0:16, i * 128:(i + 1) * 128], wlr[:, kc, i * 16:(i + 1) * 16], src3[:, kc, t0:t0 + 128],
                   kc == 0, kc == 7, [b_src, b_wlr], [b_pD])
        act(v_bf, pV[:], AF.Copy, [b_pV], [b_v])
        for i in range(2):
            cp("dve", lrTa[i][0:16, :], pD[0:16, i * 128:(i + 1) * 128], [b_pD], [b_lrTa[i]])
        for i in range(2):
            mm(pD[:], lrTa[i][:], wupa[i][:], True, True, [b_lrTa[i], b_wupa], [b_pD])
            act(e_tmp, pD[:], AF.Exp, [b_pD], [b_etmp], scale=-1.0)
            act(l_sb[i], e_tmp, AF.Ln, [b_etmp, b_const], [b_l[i]], bias=cst[:, 0:1])
        dirs = (0, 1) if need_b else (0,)
        for i in dirs:
            TC = TCf if i == 0 else TCb
            mm(pD[:], TC[:], l_sb[i], True, True, [b_const, b_l[i]], [b_pD])
            act(edec, pD[:], AF.Exp, [b_pD], [b_edec])
            tt("dve", kdec[i], pC[:], edec, ALU.mult, [b_pC, b_edec], [b_kdec[i]])
            for h in range(4):
                mm(pU[:, i * 4 + h:i * 4 + h + 1], l_sb[i][:, h * 128:(h + 1) * 128], cst[:, 2:3], True, True,
                   [b_l[i], b_const], [b_pU])
        nd = 8 if need_b else 4
        act(sm[:, 0:nd], pU[:, 0:nd], AF.Exp, [b_pU], [b_sm])

    def u_mm(i):
        for h in range(4):
            mm(pU[:, h * 256:(h + 1) * 256], kdec[i][:, h * 128:(h + 1) * 128], v_bf[:, h * 256:(h + 1) * 256], True, True,
               [b_kdec[i], b_v], [b_pU])

    def state_update(S, b_S, dcol, Sout=None, b_Sout=None):
        Sout = S if Sout is None else Sout
        b_Sout = b_S if b_Sout is None else b_Sout
        for h in range(4):
            stt("dve", Sout[:, h * 256:(h + 1) * 256], S[:, h * 256:(h + 1) * 256], dcol[:, h:h + 1],
                pU[:, h * 256:(h + 1) * 256], ALU.mult, ALU.add, [b_S, b_sm, b_pU], [b_Sout])

    ub0 = Gst[:, 0:1024]
    memset("dve", sctx[0], 0.0, [b_sctx[0]])
    for c in range(2):
        chunk_kvl(cT, b_cT, c * 128, True)
        u_mm(0)
        state_update(sctx[0], b_sctx[0], d_f)
        u_mm(1)
        if c == 0:
            cp("dve", ub0, pU[:], [b_pU], [b_G])
            cp("dve", sm[:, 24:28], d_b, [b_sm], [b_sm])
        else:
            cp("dve", sctx[1], pU[:], [b_pU], [b_sctx[1]])
    for h in range(4):
        stt("dve", sctx[1][:, h * 256:(h + 1) * 256], sctx[1][:, h * 256:(h + 1) * 256], sm[:, 24 + h:25 + h],
            ub0[:, h * 256:(h + 1) * 256], ALU.mult, ALU.add, [b_sctx[1], b_sm, b_G], [b_sctx[1]])
    dbg("sctx_f", sctx[0], [128, 1024], [b_sctx[0]])
    dbg("sctx_b", sctx[1], [128, 1024], [b_sctx[1]])

    checkpoint(3)
    memset("dve", S_f, 0.0, [b_Sf])
    memset("dve", sm[:, 16:20], 1.0, [b_sm])
    for c in range(NCH):
        chunk_kvl(hT, b_hT[c], c * 128, True)
        u_mm(0)
        state_update(S_f, b_Sf, d_f)
        tt("dve", sm[:, 16:20], sm[:, 16:20], d_f, ALU.mult, [b_sm], [b_sm])
        u_mm(1)
        act(R1v[:, c, :], pU[:], AF.Copy, [b_pU], [b_R1[c]])
        cp("dve", dB_all[:, c * 4:(c + 1) * 4], d_b, [b_sm], [b_dB])
        if c % 2 == 1:
            adaln_block_late(4 + c // 2)
    PW = 1028

    def state_allgather(dr, Lsrc, b_Lsrc):
        dcol = 16 if dr == 0 else 20
        dma("sp", agin_p[dr].ap()[:, 0:1024], Lsrc, [b_Lsrc], [b_agin_p[dr]])
        dma("sp", agin_p[dr].ap()[:, 1024:1028], sm[:, dcol:dcol + 4], [b_sm], [b_agin_p[dr]], accw=True)
        P.emit("pool", (lambda pc_: (lambda e: e.collective_compute(
            "AllGather", ALU.bypass, replica_groups=[[0, 1, 2, 3], [4, 5, 6, 7]],
            ins=[agin_p[pc_].ap().opt()], outs=[agout_p[pc_].ap().opt()])))(dr),
            [b_agin_p[dr]], [b_agout_p[dr]], extra_sem="cc_state%d" % dr)

    if int(os.environ.get("MK_STOP", "0")) != 31:
        state_allgather(0, S_f, b_Sf)
    memset("dve", S_b[0], 0.0, [b_Sb[0]])
    memset("dve", sm[:, 20:24], 1.0, [b_sm])
    cur = 0
    for c in range(NCH - 1, -1, -1):
        nxt = (cur + 1) % 3
        for h in range(4):
            stt("dve", S_b[nxt][:, h * 256:(h + 1) * 256], S_b[cur][:, h * 256:(h + 1) * 256],
                dB_all[:, c * 4 + h:c * 4 + h + 1], R1v[:, c, h * 256:(h + 1) * 256], ALU.mult, ALU.add,
                [b_Sb[cur], b_dB, b_R1[c]], [b_Sb[nxt]])
        act(R1v[:, c, :], S_b[cur], AF.Copy, [b_Sb[cur]], [b_R1[c]])
        cp("dve", DcB[:, c * 4:(c + 1) * 4], sm[:, 20:24], [b_sm], [b_DcB])
        tt("dve", sm[:, 20:24], sm[:, 20:24], dB_all[:, c * 4:(c + 1) * 4], ALU.mult, [b_sm, b_dB], [b_sm])
        cur = nxt
    L_b = S_b[cur]; b_Lb = b_Sb[cur]
    checkpoint(31)
    state_allgather(1, L_b, b_Lb)
    checkpoint(32)
    Sinb = S_b[(cur + 1) % 3]; b_Sinb = b_Sb[(cur + 1) % 3]
    cp("dve", S_f, sctx[0], [b_sctx[0]], [b_Sf])
    cp("dve", Sinb, sctx[1], [b_sctx[1]], [b_Sinb])
    gi = 0
    for (S, b_S, order, moff, dr) in ((S_f, b_Sf, (0, 1, 2, 3), 0, 0), (Sinb, b_Sinb, (3, 2, 1, 0), 4, 1)):
        for r in order:
            G_ = Gst2[gi % 2]; b_G_ = b_G2[gi % 2]; gi += 1
            dma("sp", G_, agout_p[dr].ap()[r * 128:(r + 1) * 128, :], [b_agout_p[dr]], [b_G_])
            mcol = cmask[:, moff + r:moff + r + 1]
            ts("dve", sm[:, 24:28], G_[:, 1024:1028], -1.0, mcol, ALU.add, ALU.mult, [b_G_, b_const], [b_sm])
            ts("dve", sm[:, 24:28], sm[:, 24:28], 1.0, None, ALU.add, None, [b_sm], [b_sm])
            for h in range(4):
                ts("dve", tmpS[:, h * 256:(h + 1) * 256], S[:, h * 256:(h + 1) * 256], sm[:, 24 + h:25 + h], None,
                   ALU.mult, None, [b_S, b_sm], [b_tmpS])
                stt("dve", S[:, h * 256:(h + 1) * 256], G_[:, h * 256:(h + 1) * 256], mcol, tmpS[:, h * 256:(h + 1) * 256],
                    ALU.mult, ALU.add, [b_G_, b_const, b_tmpS], [b_S])
    dbg("Sin_f", S_f, [128, 1024], [b_Sf])
    dbg("Sin_b", Sinb, [128, 1024], [b_Sinb])
    act(Sinb_bf[:], Sinb, AF.Copy, [b_Sinb], [b_Sinb_bf])
    P.barrier()
    cp("dve", Sf_bf, S_f, [b_Sf], [b_Sfbf])

    checkpoint(4)
    ogT = R3b[:, 0:16384].rearrange("p (k t) -> p k t", k=8)
    b_og = [P.buf("og%d" % c) for c in range(NCH)]
    def projQK(c_):
        t0_ = c_ * 128
        for (pX, b_pX, w, b_w) in ((pA, b_pA, wq, b_wq), (pB, b_pB, wk, b_wk)):
            for h in range(4):
                for kc in range(8):
                    mm(pX[:, h * 128:(h + 1) * 128], w[:, kc, h * 128:(h + 1) * 128], hT[:, kc, t0_:t0_ + 128], kc == 0, kc == 7,
                       [b_w, b_hT[c_]], [b_pX])

    projQK(0)
    for c in range(NCH):
        t0 = c * 128
        chunk_kvl(hT, b_hT[c], t0, False)
        for i in range(2):
            TI = TIf if i == 0 else TIb
            for h in range(4):
                mm(pD[:, h * 128:(h + 1) * 128], l_sb[i][:, h * 128:(h + 1) * 128], TI[:], True, True, [b_l[i], b_const], [b_pD])
            act(eb, pD[:], AF.Exp, [b_pD], [b_eb])
            act(enb, pD[:], AF.Exp, [b_pD], [b_enb], scale=-1.0)
            stt("dve", qin[i], pA[:], 128.0 ** -0.5, eb, ALU.mult, ALU.mult, [b_pA, b_eb], [b_qin[i]])
            tt("dve", kin[i], pB[:], enb, ALU.mult, [b_pB, b_enb], [b_kin[i]])
        for i, (pX, b_pX, M4) in enumerate(((pA, b_pA, Mf4), (pB, b_pB, Mb4))):
            for h in range(4):
                mm(pX[:, h * 128:(h + 1) * 128], kin[i][:, h * 128:(h + 1) * 128], qin[i][:, h * 128:(h + 1) * 128], True, True,
                   [b_kin[i], b_qin[i]], [b_pX])
            tt("dve", scm[i], pX[:], M4[:], ALU.mult, [b_pX, b_const], [b_scm[i]])
        tt("dve", qinD.rearrange("p (h t) -> p h t", h=4), qin[1].rearrange("p (h t) -> p h t", h=4),
           DcB[:, c * 4:(c + 1) * 4].unsqueeze(2).broadcast_to([128, 4, 128]), ALU.mult, [b_qin[1], b_DcB], [b_qinD])
        for h in range(4):
            o_ = pV[:, h * 256:(h + 1) * 256]
            vs = v_bf[:, h * 256:(h + 1) * 256]
            mm(o_, scm[0][:, h * 128:(h + 1) * 128], vs, True, False, [b_scm[0], b_v], [b_pV])
            mm(o_, scm[1][:, h * 128:(h + 1) * 128], vs, False, False, [b_scm[1], b_v], [b_pV])
            mm(o_, qin[0][:, h * 128:(h + 1) * 128], Sf_bf[:, h * 256:(h + 1) * 256], False, False, [b_qin[0], b_Sfbf], [b_pV])
            mm(o_, qin[1][:, h * 128:(h + 1) * 128], R1v[:, c, h * 256:(h + 1) * 256], False, False, [b_qin[1], b_R1[c]], [b_pV])
            mm(o_, qinD[:, h * 128:(h + 1) * 128], Sinb_bf[:, h * 256:(h + 1) * 256], False, True, [b_qinD, b_Sinb_bf], [b_pV])
        u_mm(0)
        state_update(S_f, b_Sf, d_f)
        act(Sf_bf, S_f, AF.Copy, [b_Sf], [b_Sfbf])
        for h in range(4):
            act(e_tmp[:, 0:256], pV[:, h * 256:(h + 1) * 256], AF.Square, [b_pV], [b_etmp, b_sm],
                accum_out=sm[:, 8 + h:9 + h])
        act(sm[:, 12:16], sm[:, 8:12], AF.Ln, [b_sm, b_const], [b_sm], scale=1.0 / 256, bias=cst[:, 1:2])
        act(sm[:, 12:16], sm[:, 12:16], AF.Exp, [b_sm], [b_sm], scale=-0.5)
        for h in range(4):
            ts("dve", on[:, h * 256:(h + 1) * 256], pV[:, h * 256:(h + 1) * 256], sm[:, 12 + h:13 + h], None, ALU.mult, None,
               [b_pV, b_sm], [b_on])
        if debug and c in (0, 15):
            dbg("on%d" % c, on, [128, 1024], [b_on])
        if c + 1 < NCH:
            projQK(c + 1)
        for j in range(8):
            tr(pU[:, j * 128:(j + 1) * 128], on[:, j * 128:(j + 1) * 128], ident[:], [b_on, b_ident], [b_pU])
        act(ogT[:, :, t0:t0 + 128], pU[:].rearrange("p (k t) -> p k t", k=8), AF.Copy, [b_pU], [b_og[c]])
    P.barrier()

    checkpoint(5)
    SC.reset()
    slots = [w4(R2, i) for i in range(4)]
    b_slot = [P.buf("slot%d" % i) for i in range(4)]
    sg = [SC.take(512, F32) for _ in range(2)]; b_sg = [P.buf(), P.buf()]
    t1 = [SC.take(512, F32) for _ in range(2)]; b_t1 = [P.buf(), P.buf()]
    t2 = [SC.take(512, F32) for _ in range(2)]; b_t2 = [P.buf(), P.buf()]
    xr = [SC.take(D, F32) for _ in range(2)]; b_xr = [P.buf(), P.buf()]
    psA = [(pA, b_pA), (pB, b_pB)]
    b_ogk = [[P.buf() for _ in range(4)] for _ in range(8)]
    cntr = [0]
    slot_rr = [0]

    def next_slot():
        i_ = slot_rr[0] % 4
        slot_rr[0] += 1
        return slots[i_], b_slot[i_]

    def proj_fm(pX, b_pX, wslot, b_w, col0, src3, b_src_list, tg):
        for kc in range(8):
            mm(pX[:], wslot[:, kc, col0:col0 + 128], src3[:, kc, tg * 512:(tg + 1) * 512], kc == 0, kc == 7,
               [b_w] + b_src_list, [b_pX])

    def hT_bufs(tg):
        return [b_hT[tg * 4 + q] for q in range(4)]

    def og_bufs(tg):
        return [b_og[tg * 4 + q] for q in range(4)]

    sG = [next_slot(), next_slot()]
    for blk in range(2):
        load_w(sG[blk][0], win_d.ap()[:, O_G + blk * 512:O_G + (blk + 1) * 512], [sG[blk][1]])
    for dvc in range(8):
        for tg in range(4):
            k = cntr[0] % 2; cntr[0] += 1
            pX, b_pX = psA[k]
            proj_fm(pX, b_pX, sG[dvc // 4][0], sG[dvc // 4][1], (dvc % 4) * 128, hT, hT_bufs(tg), tg)
            act(sg[k], pX[:], AF.Silu, [b_pX], [b_sg[k]])
            stt("dve", ogT[:, dvc, tg * 512:(tg + 1) * 512], ogT[:, dvc, tg * 512:(tg + 1) * 512], gn[:, dvc % 2:dvc % 2 + 1], sg[k],
                ALU.mult, ALU.mult, og_bufs(tg) + [b_sg[k], b_const], [b_ogk[dvc][tg]] + og_bufs(tg))
    if debug:
        ogdb = SC.take(D, F32); b_ogdb = P.buf()
        cp("dve", ogdb, ogT[:, :, 0:128], og_bufs(0), [b_ogdb])
        dbg("ogT0", ogdb, [128, D], [b_ogdb])
    mT = R1[:].rearrange("p (k t) -> p k t", k=8)
    b_mT = [[P.buf() for _ in range(4)] for _ in range(8)]
    def t1_loads(grp):
        sa, sb_ = next_slot(), next_slot()
        load_w(sa[0], wo_d.ap()[:, grp * 512:(grp + 1) * 512], [sa[1]])
        load_w(sb_[0], win_d.ap()[:, O_RG + grp * 512:O_RG + (grp + 1) * 512], [sb_[1]])
        return sa, sb_

    pre = t1_loads(0)
    P.barrier()
    for grp in range(2):
        sa, sb_ = pre if grp == 0 else t1_loads(1)
        for dq in range(4):
            dc = grp * 4 + dq
            for tg in range(4):
                proj_fm(pA, b_pA, sa[0], sa[1], dq * 128, ogT, og_bufs(tg), tg)
                proj_fm(pB, b_pB, sb_[0], sb_[1], dq * 128, hT, hT_bufs(tg), tg)
                k = cntr[0] % 2; cntr[0] += 1
                act(sg[k], pB[:], AF.Sigmoid, [b_pB], [b_sg[k]])
                tt("dve", mT[:, dc, tg * 512:(tg + 1) * 512], pA[:], sg[k], ALU.mult, [b_pA, b_sg[k]], [b_mT[dc][tg]])
    def conv_loads(grp):
        s3_ = [next_slot(), next_slot(), next_slot()]
        for i, off in enumerate((O_U, O_CG, O_BG)):
            load_w(s3_[i][0], win_d.ap()[:, off + grp * 512:off + (grp + 1) * 512], [s3_[i][1]])
        return s3_

    pre3 = conv_loads(0)
    P.barrier()
    bcT = R3b[:, 0:16384].rearrange("p (k t) -> p k t", k=8)
    b_bcT = [P.buf() for _ in range(4)]
    for grp in range(2):
        s3 = pre3 if grp == 0 else conv_loads(1)
        for cq in range(4):
            chc = grp * 4 + cq
            for tg in range(4):
                k = cntr[0] % 2; cntr[0] += 1
                proj_fm(pA, b_pA, s3[0][0], s3[0][1], cq * 128, hT, hT_bufs(tg), tg)
                proj_fm(pB, b_pB, s3[1][0], s3[1][1], cq * 128, hT, hT_bufs(tg), tg)
                proj_fm(pC, b_pC, s3[2][0], s3[2][1], cq * 128, hT, hT_bufs(tg), tg)
                act(sg[k], pA[:], AF.Copy, [b_pA], [b_sg[k]])
                tt("dve", t1[k], sg[k], pB[:], ALU.mult, [b_sg[k], b_pB], [b_t1[k]])
                act(t2[k], t1[k], AF.Copy, [b_t1[k], b_const], [b_t2[k]], scale=cwc[:, 8 + chc:9 + chc])
                cu3 = t1[k].rearrange("p (r w) -> p r w", w=64)
                ac3 = t2[k].rearrange("p (r w) -> p r w", w=64)
                stt("pool", ac3[:, :, 1:64], cu3[:, :, 0:63], cwc[:, chc:chc + 1], ac3[:, :, 1:64], ALU.mult, ALU.add,
                    [b_t1[k], b_t2[k], b_const], [b_t2[k]])
                stt("pool", ac3[:, :, 0:63], cu3[:, :, 1:64], cwc[:, 16 + chc:17 + chc], ac3[:, :, 0:63], ALU.mult, ALU.add,
                    [b_t1[k], b_t2[k], b_const], [b_t2[k]])
                tt("dve", bcT[:, chc, tg * 512:(tg + 1) * 512], t2[k], pC[:], ALU.mult, [b_t2[k], b_pC], [b_bcT[tg]])
    if debug:
        bcdb = SC.take(D, F32); b_bcdb = P.buf()
        cp("dve", bcdb, bcT[:, :, 0:128], [b_bcT[0]], [b_bcdb])
        dbg("bcT0", bcdb, [128, D], [b_bcdb])
    for grp in range(2):
        sa, sb_ = next_slot(), next_slot()
        load_w(sa[0], cwo_d.ap()[:, grp * 512:(grp + 1) * 512], [sa[1]])
        load_w(sb_[0], win_d.ap()[:, O_RC + grp * 512:O_RC + (grp + 1) * 512], [sb_[1]])
        for dq in range(4):
            dc = grp * 4 + dq
            for tg in range(4):
                proj_fm(pA, b_pA, sa[0], sa[1], dq * 128, bcT, [b_bcT[tg]], tg)
                proj_fm(pB, b_pB, sb_[0], sb_[1], dq * 128, hT, hT_bufs(tg), tg)
                k = cntr[0] % 2; cntr[0] += 1
                act(sg[k], pB[:], AF.Sigmoid, [b_pB], [b_sg[k]])
                tt("dve", t1[k], pA[:], sg[k], ALU.mult, [b_pA, b_sg[k]], [b_t1[k]])
                tt("dve", mT[:, dc, tg * 512:(tg + 1) * 512], t1[k], mT[:, dc, tg * 512:(tg + 1) * 512], ALU.add,
                   [b_t1[k], b_mT[dc][tg]], [b_mT[dc][tg]])
    sM = [next_slot(), next_slot()]
    for blk in range(2):
        load_w(sM[blk][0], mw_d.ap()[:, blk * 512:(blk + 1) * 512], [sM[blk][1]])
    bc_load(2, mod_d.ap()[0:1, 2 * D:3 * D])
    P.barrier()

    def x1_tile(tt_):
        return R0f[:, tt_ * D:(tt_ + 1) * D] if tt_ < 8 else R3f[:, (tt_ - 8) * D:(tt_ - 7) * D]

    b_x1 = [P.buf("x1_%d" % i) for i in range(NCH)]
    SC.reset()
    xr = [SC.take(D, F32) for _ in range(2)]; b_xr = [P.buf(), P.buf()]
    ND2 = 2
    hf2 = [SC.take(D, F32) for _ in range(ND2)]; b_hf2 = [P.buf() for _ in range(ND2)]
    hb2 = [SC.take(D, BF16) for _ in range(2)]; b_hb2 = [P.buf(), P.buf()]
    h2T = [SC.take(D, F32) for _ in range(ND2)]; b_h2T = [P.buf() for _ in range(ND2)]
    junk2 = SC.take(D, BF16); b_junk2 = P.buf()
    wr_sb = SC.take(8 * NE, F32); b_wr = P.buf()
    st2_ = [SC.take(16, F32) for _ in range(ND2)]; b_st2_ = [P.buf() for _ in range(ND2)]
    lg_ = [SC.take(NE, F32) for _ in range(2)]; b_lg_ = [P.buf(), P.buf()]
    affTp = [SC.take(512, F32) for _ in range(2)]; b_affTp = [P.buf(), P.buf()]
    bc_load(0, mod_d.ap()[0:1, 4 * D:5 * D])
    dma("sp", hf2[0], n2_d.ap()[0:1, :].broadcast_to([128, D]), (), [b_hf2[0]])
    stt("dve", bc_t[0][:], bc_t[0][:], 1.0, hf2[0], ALU.add, ALU.mult, [b_bc[0], b_hf2[0]], [b_bc[0]])
    bc_load(1, mod_d.ap()[0:1, 3 * D:4 * D])
    dma("sp", wr_sb.rearrange("p (k n) -> p k n", k=8), rw_d.ap().rearrange("(k p) n -> p k n", p=128), (), [b_wr])
    aff3 = aff[:].rearrange("p (t e) -> p t e", e=NE)
    pLg = [(pA, b_pA), (pC, b_pC)]
    def y_tile(c):
        k = c % 2
        dma("poolq", xr[k], x_d.ap()[c * 128:(c + 1) * 128, :], (), [b_xr[k]])
        for blk in range(2):
            for kc in range(8):
                mm(pV[:, blk * 512:(blk + 1) * 512], mT[:, kc, c * 128:(c + 1) * 128], sM[blk][0][:, kc, :], kc == 0, kc == 7,
                   [b_mT[kc][c // 4], sM[blk][1]], [b_pV])
        tt("dve", x1_tile(c), pV[:], bc_t[2][:], ALU.mult, [b_pV, b_bc[2]], [b_x1[c]])
        tt("dve", x1_tile(c), x1_tile(c), xr[k], ALU.add, [b_x1[c], b_xr[k]], [b_x1[c]])

    def n2_stage1(c):
        i = c % ND2
        i2 = c % 2
        src = x1_tile(c)
        st2, b_st2 = st2_[i], b_st2_[i]
        act(junk2, src, AF.Square, [b_x1[c]], [b_junk2, b_st2], accum_out=st2[:, 0:1])
        act(st2[:, 1:2], st2[:, 0:1], AF.Ln, [b_st2, b_const], [b_st2], scale=1.0 / D, bias=cst[:, 1:2])
        act(st2[:, 2:3], st2[:, 1:2], AF.Exp, [b_st2], [b_st2], scale=-0.5)
        stt("dve", hf2[i], src, st2[:, 2:3], bc_t[0][:], ALU.mult, ALU.mult, [b_x1[c], b_st2, b_bc[0]], [b_hf2[i]])
        tt("dve", hf2[i], hf2[i], bc_t[1][:], ALU.add, [b_hf2[i], b_bc[1]], [b_hf2[i]])
        act(hb2[i2], hf2[i], AF.Copy, [b_hf2[i]], [b_hb2[i2]])
        dma("sp", h2_d.ap()[c * 128:(c + 1) * 128, :], hb2[i2], [b_hb2[i2]], [b_h2_d], accw=True)
        for kc in range(8):
            tr(pU[:, kc * 128:(kc + 1) * 128], hf2[i][:, kc * 128:(kc + 1) * 128], ident[:], [b_hf2[i], b_ident], [b_pU])
        act(h2T[i], pU[:], AF.Copy, [b_pU], [b_h2T[i]])

    def n2_stage2(c):
        i = c % ND2
        i2 = c % 2
        st2, b_st2, lg, b_lg = st2_[i], b_st2_[i], lg_[i2], b_lg_[i2]
        pL, b_pL = pLg[i2]
        for kc in range(8):
            mm(pL[:, 0:NE], h2T[i][:, kc * 128:(kc + 1) * 128], wr_sb[:, kc * NE:(kc + 1) * NE], kc == 0, kc == 7,
               [b_h2T[i], b_wr], [b_pL])
        P.emit("dve", (lambda st_, pL_: (lambda e: e.tensor_reduce(out=st_[:, 4:5], in_=pL_[:, 0:NE], axis=AX.X, op=ALU.max)))(st2, pL),
               [b_pL], [b_st2])
        ts("dve", st2[:, 5:6], st2[:, 4:5], -1.0, None, ALU.mult, None, [b_st2], [b_st2])
        act(lg, pL[:, 0:NE], AF.Exp, [b_pL, b_st2], [b_lg, b_st2], bias=st2[:, 5:6], accum_out=st2[:, 6:7])
        P.emit("dve", (lambda st_: (lambda e: e.reciprocal(out=st_[:, 7:8], in_=st_[:, 6:7])))(st2), [b_st2], [b_st2])
        ts("dve", aff3[:, c, :], lg, st2[:, 7:8], None, ALU.mult, None, [b_lg, b_st2], [b_aff])
        tr(pB[0:NE, (c % 4) * 128:(c % 4 + 1) * 128], aff3[:, c, :], ident[:], [b_aff, b_ident], [b_pB])
        if c % 4 == 3:
            g_ = c // 4
            cp("dve", affTp[g_ % 2][0:NE, :], pB[0:NE, :], [b_pB], [b_affTp[g_ % 2]])
            dma("sp", affin_d.ap()[:, g_ * 512:(g_ + 1) * 512], affTp[g_ % 2][0:NE, :], [b_affTp[g_ % 2]], [b_affin_d], accw=True)

    y_tile(0)
    y_tile(1)
    n2_stage1(0)
    for c in range(NCH):
        if c + 2 < NCH:
            y_tile(c + 2)
        if c + 1 < NCH:
            n2_stage1(c + 1)
        n2_stage2(c)
    dbg("x1_0", x1_tile(0), [128, D], [b_x1[0]])
    dbg("x1_15", x1_tile(15), [128, D], [b_x1[15]])
    P.barrier()

    wd_sb = R1[:].rearrange("p (k n) -> p k n", k=16)
    b_wd = P.buf()
    gu = [[w4(R2, 0), w4(R2, 1)], [w4(R2, 2), w4(R2, 3)]]
    b_gu = [[P.buf(), P.buf()], [P.buf(), P.buf()]]

    def load_A(e, fb):
        s_ = fb % 2
        load_w(gu[s_][0], wg_d.ap()[e][:, fb * 512:(fb + 1) * 512], [b_gu[s_][0]])
        load_w(gu[s_][1], wu_d.ap()[e][:, fb * 512:(fb + 1) * 512], [b_gu[s_][1]])

    def load_wd(e):
        P.emit("pool", (lambda e__: (lambda e_: e_.dma_start(
            out=wd_sb, in_=wd_d.ap()[e__].rearrange("(k p) n -> p k n", p=128))))(e), (), [b_wd], dma=True)

    if int(os.environ.get("MK_STOP", "0")) in (0, 8):
        load_A(0, 0)
        load_A(0, 1)
        load_wd(0)
    checkpoint(6)
    dbg("aff", aff[:], [128, NCH * NE], [b_aff])
    P.emit("pool", lambda e: e.collective_compute("AllGather", ALU.bypass, replica_groups=[[0, 1, 2, 3], [4, 5, 6, 7]],
                                                 ins=[affin_d.ap().opt()], outs=[affout_d.ap().opt()]),
           [b_affin_d], [b_affout_d], extra_sem="cc_aff")
    P.barrier()
    SC.reset()
    pos = SC.take(NCH * NE, F32); b_pos = P.buf()
    L5 = SC.take(NCH * NE * 5, BF16); b_L5 = P.buf()
    colsb = [SC.take(16, F32) for _ in range(2)]; b_cols = [P.buf(), P.buf()]
    idxc = [SC.take(4, I32) for _ in range(2)]
    gatec = [SC.take(4, F32) for _ in range(2)]
    ffn_mark = SC.off
    bis = SC.take(1024, F32); b_bis = P.buf()
    bj = SC.take(1024, BF16); b_bj = P.buf()
    bs = SC.take(16, F32); b_bs = P.buf()
    for r in range(4):
        for hh in range(2):
            cidx = r * 2 + hh
            dma("sp", bis[cidx * 16:(cidx + 1) * 16, :], affout_d.ap()[r * 16:(r + 1) * 16, hh * 1024:(hh + 1) * 1024],
                [b_affout_d], [b_bis], accw=True)
    memset("dve", bs[:, 1:2], 0.5, [b_bs])
    for it in range(NBIS):
        stepn = 2.0 ** -(it + 2)
        ts("dve", bj, bis, bs[:, 1:2], None, ALU.is_ge, ALU.add, [b_bis, b_bs], [b_bj, b_bs], accum=bs[:, 0:1])
        mm(pA[:, 0:1], Gm[:], bs[:, 0:1], True, True, [b_const, b_bs], [b_pA])
        ts("dve", bs[:, 2:3], pA[:, 0:1], KTOP - 0.5, 2.0 * stepn, ALU.is_ge, ALU.mult, [b_pA], [b_bs])
        stt("dve", bs[:, 1:2], bs[:, 2:3], -stepn, bs[:, 1:2], ALU.add, ALU.add, [b_bs], [b_bs])
    ts("dve", bs[:, 3:4], bs[:, 1:2], -(2.0 ** -(NBIS + 1)), None, ALU.add, None, [b_bs], [b_bs])
    dbg("thr", bs[:, 3:4], [128, 1], [b_bs])
    thrB = SC.take(NE, F32); b_thrB = P.buf()
    thr_row = SC.take(NE, F32)
    tr(pA[0:1, 0:NE], bs[0:NE, 3:4], ident[0:NE, 0:NE], [b_bs, b_ident], [b_pA])
    cp("dve", thr_row[0:1, :], pA[0:1, 0:NE], [b_pA], [b_thrB])
    mm(pA[:, 0:NE], ones_f[0:1, :], thr_row[0:1, :], True, True, [b_thrB, b_const], [b_pA])
    cp("dve", thrB, pA[:, 0:NE], [b_pA], [b_thrB])
    mask = SC.take(NCH * NE, F32); b_mask = P.buf()
    offs = SC.take(NCH * NE, F32); b_offs = P.buf()
    mask3 = mask.rearrange("p (t e) -> p t e", e=NE)
    for c in range(NCH):
        tt("dve", mask3[:, c, :], aff3[:, c, :], thrB, ALU.is_ge, [b_aff, b_thrB], [b_mask])
    mm(pA[:, 0:256], SLT[:], mask, True, True, [b_const, b_mask], [b_pA])
    mm(pB[:, 0:256], ones_f[:], mask, True, True, [b_const, b_mask], [b_pB])
    offs3 = offs.rearrange("p (t e) -> p t e", e=NE)
    memset("dve", offs3[:, 0, :], 0.0, [b_offs])
    for c in range(1, NCH):
        tt("dve", offs3[:, c, :], offs3[:, c - 1, :], pB[:, (c - 1) * NE:c * NE], ALU.add, [b_offs, b_pB], [b_offs])
    tt("dve", pos, pA[:, 0:256], offs, ALU.add, [b_pA, b_offs], [b_pos])
    stt("dve", pos, pos, 1.0, mask, ALU.add, ALU.mult, [b_pos, b_mask], [b_pos])
    ts("dve", pos, pos, -1.0, None, ALU.add, None, [b_pos], [b_pos])
    dbg("posm", pos, [128, NCH * NE], [b_pos])
    L5v = L5.rearrange("p (t e f) -> p t e f", e=NE, f=5)
    ahi = SC.take(NCH * NE, BF16); ahif = SC.take(NCH * NE, F32); b_ah = P.buf()
    memset("dve", L5, 1.0, [b_L5])
    for c in range(NCH):
        cp("dve", L5v[:, c, :, 0], pcol[:, 0:1].broadcast_to([128, NE]), [b_const], [b_L5])
        memset("dve", L5v[:, c, :, 1], 128.0 * c, [b_L5])
    cp("dve", ahi, aff[:], [b_aff], [b_ah])
    cp("dve", ahif, ahi, [b_ah], [b_ah])
    cp("dve", L5v[:, :, :, 3], ahi.rearrange("p (t e) -> p t e", e=NE), [b_ah], [b_L5])
    tt("dve", L5v[:, :, :, 4], aff3, ahif.rearrange("p (t e) -> p t e", e=NE), ALU.subtract, [b_aff, b_ah], [b_L5])
    P.barrier()

    checkpoint(7)
    SC.off = ffn_mark
    Pe = [SC.take(CAP, BF16) for _ in range(4)]; b_Pe = [P.buf() for _ in range(4)]
    rows = SC.take(CAP, F32); b_rows = P.buf()
    xe = SC.take(3 * D, BF16); b_xe = P.buf()
    xeT = SC.take(8 * CAP, BF16); b_xeT = P.buf()
    hidT = SC.take(16 * CAP, BF16); b_hid = P.buf()
    sgl = [SC.take(CAP, F32) for _ in range(2)]; b_sgl = [P.buf(), P.buf()]
    yg = bc_t[0][:]; b_yg = b_bc[0]
    yg2 = [bc_t[0][:], bc_t[1][:]]; b_yg2 = [b_bc[0], b_bc[1]]
    ygc = [0]
    pT = pU[:].bitcast(BF16)
    b_pT = b_pU
    pG = [(pA, b_pA), (pB, b_pB)]
    pUp = [(pC, b_pC), (pD, b_pD)]
    b_pY = [P.buf(), P.buf()]
    xeT3 = xeT.rearrange("p (k s) -> p k s", k=8)
    hid3 = hidT.rearrange("p (f s) -> p f s", f=16)
    fbc = [0]

    def expert_lists(e):
        k = e % 2
        for c in range(NCH):
            q = c % 4
            ts("dve", Pe[q], iota_s[:], pos[:, c * NE + e:c * NE + e + 1], None, ALU.is_equal, None,
               [b_const, b_pos], [b_Pe[q]])
            mm(pV[0:5, 512:512 + CAP], L5v[:, c, e, :], Pe[q], c == 0, c == NCH - 1, [b_L5, b_Pe[q]], [b_pY[1]])
        cp("dve", rows[0:5, :], pV[0:5, 512:512 + CAP], [b_pY[1]], [b_rows])
        dma("sp", row_d.ap()[e], rows[0:5, :], [b_rows], [b_row_d[e]])
        c3 = colsb[k][:, 0:15].rearrange("p (f j) -> p f j", j=3)
        dma("sp", c3, row_d.ap()[e].rearrange("f (p j) -> p f j", j=3), [b_row_d[e]], [b_cols[k]])
        tt("dve", gatec[k][:, 0:3], c3[:, 3, :], c3[:, 4, :], ALU.add, [b_cols[k]], [b_cols[k]])
        tt("dve", c3[:, 0, :], c3[:, 0, :], c3[:, 1, :], ALU.add, [b_cols[k]], [b_cols[k]])
        ts("dve", c3[:, 2, :], c3[:, 2, :], -1.0, -1.0, ALU.add, ALU.mult, [b_cols[k]], [b_cols[k]])
        tt("dve", c3[:, 2, :], c3[:, 2, :], basecol[:], ALU.mult, [b_cols[k], b_const], [b_cols[k]])
        tt("dve", c3[:, 0, :], c3[:, 0, :], c3[:, 2, :], ALU.add, [b_cols[k]], [b_cols[k]])
        cp("dve", idxc[k][:, 0:3], c3[:, 0, :], [b_cols[k]], [b_cols[k]])
        if debug and e == 0:
            dbg("idx0", c3[:, 0, :], [128, 3], [b_cols[k]])
            dbg("gate0", gatec[k][:, 0:3], [128, 3], [b_cols[k]])
        for j in range(3):
            P.emit("pool", (lambda k_, j_: (lambda e_: e_.indirect_dma_start(
                out=xe[:, j_ * D:(j_ + 1) * D], out_offset=None, in_=h2_d.ap(),
                in_offset=bass.IndirectOffsetOnAxis(ap=idxc[k_][:, j_:j_ + 1], axis=0))))(k, j),
                [b_cols[k], b_h2_d], [b_xe], dma=True, accw=(j > 0))

    def tr_b(out, in_, rd, wr):
        P.emit("pe", lambda e_: e_.transpose(out=out, in_=in_, identity=ident_b[:]), rd, wr)

    expert_lists(0)
    for e in range(NE):
        k = e % 2
        for j in range(3):
            for kc in range(8):
                tr_b(pT[:, kc * 128:(kc + 1) * 128], xe[:, j * D + kc * 128:j * D + (kc + 1) * 128], [b_xe, b_ident], [b_pT])
            act(xeT3[:, :, j * 128:(j + 1) * 128], pT[:, 0:1024].rearrange("p (k s) -> p k s", k=8), AF.Copy, [b_pT], [b_xeT])
        for fb in range(4):
            s_ = fb % 2
            for fc in range(4):
                kk = fc % 2
                pg, b_pg = pG[kk]
                pu, b_pu = pUp[kk]
                for kc in range(8):
                    mm(pg[:, 0:CAP], gu[s_][0][:, kc, fc * 128:(fc + 1) * 128], xeT3[:, kc, :], kc == 0, kc == 7,
                       [b_gu[s_][0], b_xeT], [b_pg])
                for kc in range(8):
                    mm(pu[:, 0:CAP], gu[s_][1][:, kc, fc * 128:(fc + 1) * 128], xeT3[:, kc, :], kc == 0, kc == 7,
                       [b_gu[s_][1], b_xeT], [b_pu])
                act(sgl[kk], pg[:, 0:CAP], AF.Silu, [b_pg], [b_sgl[kk]])
                tt("dve", hid3[:, fb * 4 + fc, :], sgl[kk], pu[:, 0:CAP], ALU.mult, [b_sgl[kk], b_pu], [b_hid])
            if fb + 2 < 4:
                load_A(e, fb + 2)
            if fb == 1 and e + 1 < NE:
                expert_lists(e + 1)
        if e + 1 < NE:
            load_A(e + 1, 0)
            load_A(e + 1, 1)
        for j in range(3):
            for half in range(2):
                for f in range(16):
                    mm(pV[:, half * 512:(half + 1) * 512], hid3[:, f, j * 128:(j + 1) * 128],
                       wd_sb[:, f, half * 512:(half + 1) * 512], f == 0, f == 15, [b_hid, b_wd], [b_pY[half]])
            ygi = ygc[0] % 2; ygc[0] += 1
            yg_, b_yg_ = yg2[ygi], b_yg2[ygi]
            act(yg_[:, 0:512], pV[:, 0:512], AF.Copy, [b_pY[0], b_cols[k]], [b_yg_], scale=gatec[k][:, j:j + 1])
            ts("dve", yg_[:, 512:1024], pV[:, 512:1024], gatec[k][:, j:j + 1], None, ALU.mult, None,
               [b_pY[1], b_cols[k], b_yg_], [b_yg_])
            P.emit("pool", (lambda k_, j_, yg__: (lambda e_: e_.indirect_dma_start(
                out=acc_d.ap(), out_offset=bass.IndirectOffsetOnAxis(ap=idxc[k_][:, j_:j_ + 1], axis=0),
                in_=yg__, in_offset=None, compute_op=ALU.add)))(k, j, yg_),
                [b_yg_, b_cols[k], b_acc_d], [b_acc_d], dma=True)
        if e + 1 < NE:
            load_wd(e + 1)
    P.barrier()

    checkpoint(8)
    SC.reset()
    bc_load(2, mod_d.ap()[0:1, 5 * D:6 * D])
    bc_load(0, fg_d.ap()[0:1, :])
    ac = [SC.take(D, F32) for _ in range(4)]; b_ac = [P.buf() for _ in range(4)]
    ot = [SC.take(D, F32) for _ in range(2)]; b_ot = [P.buf(), P.buf()]
    junk3 = [SC.take(D, BF16) for _ in range(2)]; b_junk3 = [P.buf(), P.buf()]
    st3 = [SC.take(8, F32) for _ in range(2)]; b_st3 = [P.buf(), P.buf()]
    def fin_load(c_):
        dma("poolq", ac[c_ % 4], acc_d.ap()[c_ * 128:(c_ + 1) * 128, :], [b_acc_d], [b_ac[c_ % 4]])

    for c_ in range(3):
        fin_load(c_)
    for c in range(NCH):
        k = c % 4
        k2 = c % 2
        if c + 3 < NCH:
            fin_load(c + 3)
        tt("dve", ac[k], ac[k], bc_t[2][:], ALU.mult, [b_ac[k], b_bc[2]], [b_ac[k]])
        tt("dve", ac[k], ac[k], x1_tile(c), ALU.add, [b_ac[k], b_x1[c]], [b_ac[k]])
        act(junk3[k2], ac[k], AF.Square, [b_ac[k]], [b_junk3[k2], b_st3[k2]], accum_out=st3[k2][:, 0:1])
        act(st3[k2][:, 1:2], st3[k2][:, 0:1], AF.Ln, [b_st3[k2], b_const], [b_st3[k2]], scale=1.0 / D, bias=cst[:, 1:2])
        act(st3[k2][:, 2:3], st3[k2][:, 1:2], AF.Exp, [b_st3[k2]], [b_st3[k2]], scale=-0.5)
        stt("dve", ot[k2], ac[k], st3[k2][:, 2:3], bc_t[0][:], ALU.mult, ALU.mult, [b_ac[k], b_st3[k2], b_bc[0]], [b_ot[k2]])
        dma("sp", out_d.ap()[c * 128:(c + 1) * 128, :], ot[k2], [b_ot[k2]], [b_out_d], accw=True)


_CACHE = {}


def _NWE():
    return 1 if int(os.environ.get("MK_STOP", "0")) not in (0, 7, 8) else NE


def make_maps(inp):
    f32 = lambda a: np.ascontiguousarray(np.asarray(a, dtype=np.float32))
    x = f32(inp["x"]); c = f32(inp["c"]); ctx = f32(inp["ctx"]); c_ctx = f32(inp["c_ctx"])
    shared = {
        "w_ada": f32(inp["w_ada"][0]),
        "b_ada": f32(inp["b_ada"][0]).reshape(1, 6 * D),
        "norm1_g": f32(inp["norm1_g"][0]).reshape(1, D),
        "norm2_g": f32(inp["norm2_g"][0]).reshape(1, D),
        "w_in": f32(inp["w_in"][0]),
        "w_a_up": f32(inp["gla_w_a_up"][0]).reshape(32, 512),
        "b_a": f32(inp["gla_b_a"][0]).reshape(2, 512),
        "gn_col": f32(f32(inp["gla_norm_g"][0]).reshape(2, 128).T),
        "gla_w_o": f32(inp["gla_w_o"][0]),
        "cw_col": f32(f32(inp["conv_w"][0]).reshape(3, 8, 128).transpose(2, 0, 1).reshape(128, 24)),
        "conv_w_out": f32(inp["conv_w_out"][0]),
        "merge_w_out": f32(inp["merge_w_out"][0]),
        "router_w": f32(inp["router_w"][0]),
        "exp_w_gate": f32(inp["exp_w_gate"][0][:_NWE()]),
        "exp_w_up": f32(inp["exp_w_up"][0][:_NWE()]),
        "exp_w_down": f32(inp["exp_w_down"][0][:_NWE()]),
        "final_g": f32(inp["final_g"]).reshape(1, D),
    }
    maps = []
    for core in range(8):
        b, r = divmod(core, 4)
        cc = np.concatenate([c[b].reshape(8, 128).T, c_ctx.reshape(8, 128).T], axis=1)
        cm = np.zeros((128, 8), np.float32)
        for q in range(4):
            cm[:, q] = 1.0 if q < r else 0.0
            cm[:, 4 + q] = 1.0 if q > r else 0.0
        m = dict(shared)
        m["x"] = f32(x[b, r * NT:(r + 1) * NT])
        m["ctx"] = f32(ctx[b])
        m["cc"] = f32(cc)
        m["cmask"] = cm
        maps.append(m)
    return maps


def kernel(**inputs):
    nc = build_program(debug=False)[0]
    maps = make_maps(inputs)
    res = run_bass_kernel_spmd(nc, maps, core_ids=list(range(8)))
    out = np.empty((2, 4 * NT, D), np.float32)
    for core in range(8):
        b, r = divmod(core, 4)
        out[b, r * NT:(r + 1) * NT] = np.asarray(res.results[core]["out"], dtype=np.float32)
    return out
```
